# Optimizing a Trainium2 kernel written in Bass

```python
import math
import jax, jax.numpy as jnp
from jax import lax
import numpy as np

D_MODEL = 1024
BATCH = 8
SEQ = 2048
DEPTH = 4

CHUNK = 64
N_MIXERS = 3
D_FF = 4 * D_MODEL
NORM_EPS = 1e-6
GDN_HEADS = 8
GDN_DK = D_MODEL // GDN_HEADS
GDN_DV = D_MODEL // GDN_HEADS
GDN_CONV = 4
GDN_QKV = GDN_HEADS * (2 * GDN_DK + GDN_DV)
GDN_IN = GDN_QKV + GDN_HEADS * GDN_DV + 2 * GDN_HEADS
MLSTM_HEADS = 8
MLSTM_DV = D_MODEL // MLSTM_HEADS
MLSTM_DK = MLSTM_DV // 2
GATE_CAP = 15.0
MLSTM_IN = MLSTM_HEADS * (2 * MLSTM_DK + 2 * MLSTM_DV) + 2 * MLSTM_HEADS
DIFF_HEADS = 8
DIFF_D = D_MODEL // (2 * DIFF_HEADS)
ROPE_THETA = 500000.0
ROT_DIMS = DIFF_D // 4
Q_BLOCK = 128
N_GDN = (DEPTH + 2) // 3
N_MLSTM = (DEPTH + 1) // 3
N_DIFF = DEPTH // 3

kernel_name = "hybrid_gdn_mlstm_diffattn_trunk"


def rms_norm(x, g):
    xf = x.astype(jnp.float32)
    y = xf * lax.rsqrt(jnp.mean(xf * xf, axis=-1, keepdims=True) + NORM_EPS)
    return (y * g.astype(jnp.float32)).astype(x.dtype)


def l2_norm(x):
    xf = x.astype(jnp.float32)
    return xf * lax.rsqrt(jnp.sum(xf * xf, axis=-1, keepdims=True) + NORM_EPS)


def soft_cap(x):
    return GATE_CAP * jnp.tanh(x / GATE_CAP)


def to_chunks(x):
    b, t, h = x.shape[:3]
    x = x.reshape((b, t // CHUNK, CHUNK, h) + x.shape[3:])
    return jnp.swapaxes(jnp.moveaxis(x, 1, 0), 2, 3)


def from_chunks(x):
    n, b, h, c, d = x.shape
    return jnp.moveaxis(jnp.swapaxes(x, 2, 3), 0, 1).reshape(b, n * c, h, d)


def causal_conv(x, w):
    width, t = w.shape[0], x.shape[1]
    xp = jnp.pad(x, ((0, 0), (width - 1, 0), (0, 0)))
    return sum(xp[:, j:j + t] * w[j] for j in range(width))


def partial_rope(x, cos, sin):
    shp = (1, x.shape[1]) + (1,) * (x.ndim - 3) + (ROT_DIMS // 2,)
    c, s = cos.reshape(shp), sin.reshape(shp)
    x1, x2, rest = x[..., :ROT_DIMS // 2], x[..., ROT_DIMS // 2:ROT_DIMS], x[..., ROT_DIMS:]
    return jnp.concatenate([x1 * c - x2 * s, x1 * s + x2 * c, rest], axis=-1)


def gated_delta_rule(q, k, v, g, beta):
    dk, dv = q.shape[-1], v.shape[-1]
    qc = to_chunks(q * dk ** -0.5)
    kc = to_chunks(k)
    vc = to_chunks(v.astype(jnp.float32))
    gc = jnp.cumsum(to_chunks(g.astype(jnp.float32)), axis=-1)
    bc = to_chunks(beta.astype(jnp.float32))
    tril = jnp.tril(jnp.ones((CHUNK, CHUNK), bool))
    strict = jnp.tril(jnp.ones((CHUNK, CHUNK), bool), -1)
    diff = gc[..., :, None] - gc[..., None, :]
    decay = jnp.where(tril, jnp.exp(jnp.where(tril, diff, 0.0)), 0.0)
    kb = kc * bc[..., None]
    a_mat = jnp.where(strict, jnp.einsum('nbhid,nbhjd->nbhij', kb, kc) * decay, 0.0)
    t_mat = a_mat + jnp.eye(CHUNK, dtype=jnp.float32)
    rhs = jnp.concatenate([vc * bc[..., None], kb * jnp.exp(gc)[..., None]], axis=-1)
    sol = lax.linalg.triangular_solve(t_mat, rhs, left_side=True, lower=True, unit_diagonal=True)
    value, kcd = sol[..., :dv], sol[..., dv:]
    attn = jnp.einsum('nbhid,nbhjd->nbhij', qc, kc) * decay
    q_inter = qc * jnp.exp(gc)[..., None]
    g_last = gc[..., -1]
    k_state = kc * jnp.exp(g_last[..., None] - gc)[..., None]

    def step(s_st, xs):
        qi, kcd_i, val_i, attn_i, kst_i, gl_i = xs
        v_new = val_i - jnp.einsum('bhck,bhkv->bhcv', kcd_i, s_st)
        o = jnp.einsum('bhck,bhkv->bhcv', qi, s_st) + jnp.einsum('bhcs,bhsv->bhcv', attn_i, v_new)
        s_st = s_st * jnp.exp(gl_i)[..., None, None] + jnp.einsum('bhck,bhcv->bhkv', kst_i, v_new)
        return s_st, o

    s0 = jnp.zeros(qc.shape[1:3] + (dk, dv), jnp.float32)
    _, o = lax.scan(step, s0, (q_inter, kcd, value, attn, k_state, g_last))
    return from_chunks(o)


def gdn_mixer(h, w_in, conv_w, a_log, dt_bias, norm_g, w_out):
    b, t, _ = h.shape
    proj = h @ w_in
    qkv = jax.nn.silu(causal_conv(proj[..., :GDN_QKV], conv_w))
    z = proj[..., GDN_QKV:GDN_QKV + GDN_HEADS * GDN_DV].reshape(b, t, GDN_HEADS, GDN_DV)
    a = proj[..., GDN_QKV + GDN_HEADS * GDN_DV:GDN_IN - GDN_HEADS]
    bt = proj[..., GDN_IN - GDN_HEADS:]
    hk = GDN_HEADS * GDN_DK
    q = l2_norm(qkv[..., :hk].reshape(b, t, GDN_HEADS, GDN_DK))
    k = l2_norm(qkv[..., hk:2 * hk].reshape(b, t, GDN_HEADS, GDN_DK))
    v = qkv[..., 2 * hk:].reshape(b, t, GDN_HEADS, GDN_DV)
    g = -jnp.exp(a_log.astype(jnp.float32)) * jax.nn.softplus((a + dt_bias).astype(jnp.float32))
    beta = jax.nn.sigmoid(bt.astype(jnp.float32))
    o = gated_delta_rule(q, k, v, g, beta)
    o = rms_norm(o, norm_g) * jax.nn.silu(z.astype(jnp.float32))
    return o.reshape(b, t, GDN_HEADS * GDN_DV).astype(h.dtype) @ w_out


def mlstm_chunkwise(q, k, v, i_pre, f_pre):
    dk, dv = q.shape[-1], v.shape[-1]
    qc = to_chunks(q.astype(jnp.float32) * dk ** -0.5)
    kc = to_chunks(k.astype(jnp.float32))
    vc = to_chunks(v.astype(jnp.float32))
    lfc = to_chunks(jax.nn.log_sigmoid(f_pre.astype(jnp.float32)))
    ic = to_chunks(i_pre.astype(jnp.float32))
    tril = jnp.tril(jnp.ones((CHUNK, CHUNK), bool))

    def step(carry, xs):
        c_st, n_st, m_st = carry
        q_, k_, v_, lf, ig = xs
        bcum = jnp.cumsum(lf, axis=-1)
        g = bcum[..., -1]
        dlog = jnp.where(tril, bcum[..., :, None] - bcum[..., None, :] + ig[..., None, :], -jnp.inf)
        m_inter = bcum + m_st[..., None]
        m_out = jnp.maximum(m_inter, jnp.max(dlog, axis=-1))
        w_inter = jnp.exp(m_inter - m_out)
        s = jnp.einsum('bhtd,bhsd->bhts', q_, k_) * jnp.exp(dlog - m_out[..., None])
        num = w_inter[..., None] * jnp.einsum('bhtd,bhde->bhte', q_, c_st) + jnp.einsum('bhts,bhse->bhte', s, v_)
        den = w_inter * jnp.einsum('bhtd,bhd->bht', q_, n_st) + jnp.sum(s, axis=-1)
        h_ = num / jnp.maximum(jnp.abs(den), jnp.exp(-m_out))[..., None]
        a = g[..., None] - bcum + ig
        m_new = jnp.maximum(g + m_st, jnp.max(a, axis=-1))
        w_old = jnp.exp(g + m_st - m_new)
        kw = k_ * jnp.exp(a - m_new[..., None])[..., None]
        c_new = w_old[..., None, None] * c_st + jnp.einsum('bhsd,bhse->bhde', kw, v_)
        n_new = w_old[..., None] * n_st + jnp.sum(kw, axis=-2)
        return (c_new, n_new, m_new), h_

    bh = qc.shape[1:3]
    init = (jnp.zeros(bh + (dk, dv), jnp.float32), jnp.zeros(bh + (dk,), jnp.float32),
            jnp.zeros(bh, jnp.float32))
    _, hs = lax.scan(step, init, (qc, kc, vc, lfc, ic))
    return from_chunks(hs)


def mlstm_mixer(h, w_in, gate_b, norm_g, w_out):
    b, t, _ = h.shape
    proj = h @ w_in
    hk, hv = MLSTM_HEADS * MLSTM_DK, MLSTM_HEADS * MLSTM_DV
    q = proj[..., :hk].reshape(b, t, MLSTM_HEADS, MLSTM_DK)
    k = proj[..., hk:2 * hk].reshape(b, t, MLSTM_HEADS, MLSTM_DK)
    v = proj[..., 2 * hk:2 * hk + hv].reshape(b, t, MLSTM_HEADS, MLSTM_DV)
    o_gate = proj[..., 2 * hk + hv:2 * hk + 2 * hv]
    i_pre = soft_cap(proj[..., MLSTM_IN - 2 * MLSTM_HEADS:MLSTM_IN - MLSTM_HEADS] + gate_b[0])
    f_pre = soft_cap(proj[..., MLSTM_IN - MLSTM_HEADS:] + gate_b[1])
    hc = mlstm_chunkwise(q, k, v, i_pre, f_pre)
    hn = rms_norm(hc, norm_g.reshape(MLSTM_HEADS, MLSTM_DV)).reshape(b, t, hv)
    return (jax.nn.sigmoid(o_gate.astype(jnp.float32)) * hn).astype(h.dtype) @ w_out


def diff_attention(q, k, v, lam):
    b, t, h, _, d = q.shape
    nq = t // Q_BLOCK
    qf = jnp.moveaxis((q.astype(jnp.float32) * d ** -0.5).reshape(b, nq, Q_BLOCK, h, 2, d), 1, 0)
    kf, vf = k.astype(jnp.float32), v.astype(jnp.float32)
    key_chunk = jnp.arange(t) // CHUNK

    def one_block(args):
        qb, idx = args
        q_chunk = (idx * Q_BLOCK + jnp.arange(Q_BLOCK)) // CHUNK
        s = jnp.einsum('bqhcd,bkhcd->bhcqk', qb, kf)
        mask = key_chunk[None, :] <= q_chunk[:, None]
        p = jax.nn.softmax(jnp.where(mask, s, -jnp.inf), axis=-1)
        attn = p[:, :, 0] - lam * p[:, :, 1]
        return jnp.einsum('bhqk,bkhe->bqhe', attn, vf)

    o = lax.map(one_block, (qf, jnp.arange(nq)))
    return jnp.moveaxis(o, 0, 1).reshape(b, t, h, 2 * d)


def diff_mixer(h, w_in, lam_p, norm_g, w_out, lambda_init, cos, sin):
    b, t, _ = h.shape
    proj = h @ w_in
    hq = DIFF_HEADS * 2 * DIFF_D
    q = partial_rope(proj[..., :hq].reshape(b, t, DIFF_HEADS, 2, DIFF_D), cos, sin)
    k = partial_rope(proj[..., hq:2 * hq].reshape(b, t, DIFF_HEADS, 2, DIFF_D), cos, sin)
    v = proj[..., 2 * hq:].reshape(b, t, DIFF_HEADS, 2 * DIFF_D)
    lp = lam_p.astype(jnp.float32)
    lam = jnp.exp(jnp.sum(lp[0] * lp[1])) - jnp.exp(jnp.sum(lp[2] * lp[3])) + lambda_init
    o = diff_attention(q, k, v, lam)
    o = rms_norm(o, norm_g) * (1.0 - lambda_init)
    return o.reshape(b, t, hq).astype(h.dtype) @ w_out


def sq_relu_mlp(h, w1, w2):
    return jnp.square(jax.nn.relu(h @ w1)) @ w2


def setup_inputs(seed: int = 0) -> dict:
    key = jax.random.key(seed)
    ks = jax.random.split(key, 18)
    nrm = lambda k, shape, scale: jax.random.normal(k, shape, jnp.float32) * scale
    return {
        "x": nrm(ks[0], (BATCH, SEQ, D_MODEL), 1.0),
        "norm_g": 1.0 + nrm(ks[1], (DEPTH, 4, D_MODEL), 0.05),
        "mlp_w1": nrm(ks[2], (DEPTH, D_MODEL, D_FF), D_MODEL ** -0.5),
        "mlp_w2": nrm(ks[3], (DEPTH, D_FF, D_MODEL), D_FF ** -0.5),
        "gdn_w_in": nrm(ks[4], (N_GDN, D_MODEL, GDN_IN), D_MODEL ** -0.5),
        "gdn_conv": nrm(ks[5], (N_GDN, GDN_CONV, GDN_QKV), GDN_CONV ** -0.5),
        "gdn_a_log": jnp.log(jax.random.uniform(ks[6], (N_GDN, GDN_HEADS), jnp.float32, 1.0, 16.0)),
        "gdn_dt_bias": -4.0 + nrm(ks[7], (N_GDN, GDN_HEADS), 0.5),
        "gdn_norm_g": 1.0 + nrm(ks[8], (N_GDN, GDN_DV), 0.05),
        "gdn_w_out": nrm(ks[9], (N_GDN, GDN_HEADS * GDN_DV, D_MODEL), (GDN_HEADS * GDN_DV) ** -0.5),
        "mlstm_w_in": nrm(ks[10], (N_MLSTM, D_MODEL, MLSTM_IN), D_MODEL ** -0.5),
        "mlstm_gate_b": jnp.array([[-1.0], [3.0]], jnp.float32) + nrm(ks[11], (N_MLSTM, 2, MLSTM_HEADS), 0.1),
        "mlstm_norm_g": 1.0 + nrm(ks[12], (N_MLSTM, MLSTM_HEADS * MLSTM_DV), 0.05),
        "mlstm_w_out": nrm(ks[13], (N_MLSTM, MLSTM_HEADS * MLSTM_DV, D_MODEL), (MLSTM_HEADS * MLSTM_DV) ** -0.5),
        "diff_w_in": nrm(ks[14], (N_DIFF, D_MODEL, 3 * DIFF_HEADS * 2 * DIFF_D), D_MODEL ** -0.5),
        "diff_lambda": nrm(ks[15], (N_DIFF, 4, DIFF_D), 0.1),
        "diff_norm_g": 1.0 + nrm(ks[16], (N_DIFF, 2 * DIFF_D), 0.05),
        "diff_w_out": nrm(ks[17], (N_DIFF, DIFF_HEADS * 2 * DIFF_D, D_MODEL), (DIFF_HEADS * 2 * DIFF_D) ** -0.5),
    }


def reference(x, norm_g, mlp_w1, mlp_w2, gdn_w_in, gdn_conv, gdn_a_log, gdn_dt_bias, gdn_norm_g,
              gdn_w_out, mlstm_w_in, mlstm_gate_b, mlstm_norm_g, mlstm_w_out, diff_w_in,
              diff_lambda, diff_norm_g, diff_w_out):
    t = x.shape[1]
    inv_freq = ROPE_THETA ** (-jnp.arange(0, ROT_DIMS, 2, dtype=jnp.float32) / ROT_DIMS)
    ang = jnp.arange(t, dtype=jnp.float32)[:, None] * inv_freq[None, :]
    cos, sin = jnp.cos(ang).astype(x.dtype), jnp.sin(ang).astype(x.dtype)
    for i in range(DEPTH):
        kind, j = i % N_MIXERS, i // N_MIXERS
        h = rms_norm(x, norm_g[i, 0])
        if kind == 0:
            m = gdn_mixer(h, gdn_w_in[j], gdn_conv[j], gdn_a_log[j], gdn_dt_bias[j], gdn_norm_g[j], gdn_w_out[j])
        elif kind == 1:
            m = mlstm_mixer(h, mlstm_w_in[j], mlstm_gate_b[j], mlstm_norm_g[j], mlstm_w_out[j])
        else:
            lambda_init = 0.8 - 0.6 * math.exp(-0.3 * i)
            m = diff_mixer(h, diff_w_in[j], diff_lambda[j], diff_norm_g[j], diff_w_out[j], lambda_init, cos, sin)
        x = x + rms_norm(m, norm_g[i, 1])
        h = rms_norm(x, norm_g[i, 2])
        x = x + rms_norm(sq_relu_mlp(h, mlp_w1[i], mlp_w2[i]), norm_g[i, 3])
    return x
```

```python
import numpy as np
import concourse.bass as bass
import concourse.mybir as mybir

F32 = mybir.dt.float32
BF = mybir.dt.bfloat16
AF = mybir.ActivationFunctionType
ALU = mybir.AluOpType
AX = mybir.AxisListType
DSZ = {F32: 4, BF: 2}

SB_CELL = 256
PS_CELL = 2048
ENGINES = ["pe", "act", "dve", "pool", "sp"]
NDMA = 24


class V:
    __slots__ = ("ap", "space", "lo", "hi")

    def __init__(self, ap, space, lo, hi):
        self.ap, self.space, self.lo, self.hi = ap, space, lo, hi


class Tn:
    def __init__(self, full_ap, space, off, shape, dtype, parts=128):
        self.full, self.space, self.off = full_ap, space, off
        self.shape, self.dtype, self.parts = tuple(shape), dtype, parts
        self.esz = DSZ[dtype]
        st, s = [], 1
        for d in reversed(self.shape):
            st.append(s)
            s *= d
        self.strides = tuple(reversed(st))
        self.nbytes = s * self.esz

    def __getitem__(self, idx):
        if not isinstance(idx, tuple):
            idx = (idx,)
        assert len(idx) == len(self.shape) + 1, (idx, self.shape)
        lo = hi = 0
        for k, (i, d, stn) in enumerate(zip(idx[1:], self.shape, self.strides)):
            if isinstance(i, slice):
                a = 0 if i.start is None else i.start
                b = d if i.stop is None else i.stop
                assert 0 <= a < b <= d, (idx, self.shape)
            else:
                a, b = i, i + 1
                assert 0 <= a < d
            lo += a * stn
            hi += (b - 1) * stn
        hi += 1
        return V(self.full[idx], self.space, self.off + lo * self.esz, self.off + hi * self.esz)

    def all(self):
        return self[(slice(None),) * (len(self.shape) + 1)]


class Sched:
    def __init__(self, nc, sb_bytes=212000):
        self.nc = nc
        self.sb_words = sb_bytes // 4
        self.arena = nc.alloc_sbuf_tensor("arena", [128, self.sb_words], F32)
        self.psum = nc.alloc_psum_tensor("psum", [128, 4096], F32)
        self.sb_top = 0
        self.ops = {e: [] for e in ENGINES}
        self.count = {e: 0 for e in ENGINES}
        self.cells = {}
        self.seen = {e: {} for e in ENGINES}
        self.dma_cnt = [0] * NDMA
        self.dma_rr = 0
        self.out_tokens = []
        self.nwaits = 0
        self.q_hist = {e: [] for e in ENGINES}
        self.q_max = {"pool": 2, "sp": 8, "act": 4}

    def sb(self, shape, dtype, parts=128):
        n = int(np.prod(shape)) * DSZ[dtype]
        n = (n + 63) // 64 * 64
        off = self.sb_top
        self.sb_top += n
        assert self.sb_top <= self.sb_words * 4, f"SBUF overflow {self.sb_top}"
        return self.sb_at(off, shape, dtype, parts)

    def sb_at(self, off, shape, dtype, parts=128):
        n = int(np.prod(shape)) * DSZ[dtype]
        ap = self.arena[0:parts, off // 4:(off + n + 3) // 4]
        if dtype != F32:
            ap = ap.bitcast(dtype)
        if len(shape) > 1:
            names = " ".join(f"d{i}" for i in range(len(shape)))
            kw = {f"d{i}": shape[i] for i in range(len(shape))}
            ap = ap.rearrange(f"p ({names}) -> p {names}", **kw)
        return Tn(ap, "sb", off, shape, dtype, parts)

    def ps(self, off, shape, dtype=F32, parts=128):
        n = int(np.prod(shape)) * DSZ[dtype]
        assert off // 2048 == (off + n - 1) // 2048, "psum tensor crosses bank"
        ap = self.psum[0:parts, off // 4:(off + n + 3) // 4]
        if dtype != F32:
            ap = ap.bitcast(dtype)
        if len(shape) > 1:
            names = " ".join(f"d{i}" for i in range(len(shape)))
            kw = {f"d{i}": shape[i] for i in range(len(shape))}
            ap = ap.rearrange(f"p ({names}) -> p {names}", **kw)
        return Tn(ap, "ps", off, shape, dtype, parts)

    def _cells(self, v):
        c = SB_CELL if v.space == "sb" else PS_CELL
        return [(v.space, i) for i in range(v.lo // c, (v.hi - 1) // c + 1)]

    def _deps(self, eng, reads, writes, is_dma):
        deps = {}

        def need(tok, same_ok):
            if tok is None:
                return
            key, val, teng = tok
            if teng == eng and same_ok and not is_dma:
                return
            if deps.get(key, 0) < val:
                deps[key] = val

        for v in reads:
            if v.space == "dram":
                continue
            for c in self._cells(v):
                st = self.cells.get(c)
                if st:
                    need(st["w"], False)
                    if v.space == "ps":
                        for r in st["r"]:
                            need(r, True)
        for v in writes:
            if v.space == "dram":
                continue
            for c in self._cells(v):
                st = self.cells.get(c)
                if st:
                    need(st["w"], True)
                    for r in st["r"]:
                        need(r, True)
        return deps

    def _commit(self, tok, reads, writes):
        for v in reads:
            if v.space == "dram":
                continue
            for c in self._cells(v):
                st = self.cells.setdefault(c, {"w": None, "r": []})
                st["r"] = [r for r in st["r"] if r[0] != tok[0]] + [tok]
        for v in writes:
            if v.space == "dram":
                continue
            for c in self._cells(v):
                self.cells[c] = {"w": tok, "r": []}

    def _prune(self, eng, deps):
        out = []
        seen = self.seen[eng]
        for key, val in deps.items():
            if key == eng:
                pass
            if seen.get(key, 0) >= val:
                continue
            seen[key] = val
            out.append((key, val))
        return out

    def op(self, eng, fn, reads=(), writes=()):
        reads = [r for r in reads if r is not None]
        deps = self._deps(eng, reads, writes, False)
        waits = self._prune(eng, deps)
        self.count[eng] += 1
        tok = (eng, self.count[eng], eng)
        self.ops[eng].append((waits, fn, (eng, 1)))
        self._commit(tok, reads, writes)
        return tok

    def dma(self, eng, fn, reads=(), writes=(), is_output=False):
        k = self.dma_rr
        self.dma_rr = (self.dma_rr + 1) % NDMA
        deps = self._deps(eng, reads, writes, True)
        key = ("dma", k)
        if self.dma_cnt[k] > 0:
            deps[key] = max(deps.get(key, 0), self.dma_cnt[k])
        hist = self.q_hist[eng]
        qm = self.q_max.get(eng, 4)
        if len(hist) >= qm:
            pk, pv, _ = hist[-qm]
            deps[pk] = max(deps.get(pk, 0), pv)
        waits = self._prune(eng, deps)
        self.dma_cnt[k] += 16
        tok = (key, self.dma_cnt[k], "dmaq")
        self.ops[eng].append((waits, fn, (key, 16)))
        self._commit(tok, reads, writes)
        hist.append(tok)
        if is_output:
            self.out_tokens.append(tok)
        return tok

    def barrier(self):
        for e in ENGINES:
            deps = {}
            for x in ENGINES:
                if x != e and self.count[x] > 0:
                    deps[x] = self.count[x]
            for k in range(NDMA):
                if self.dma_cnt[k] > 0:
                    deps[("dma", k)] = self.dma_cnt[k]
            waits = self._prune(e, deps)
            if waits:
                self.ops[e].append((waits, None, None))

    def emit(self):
        nc = self.nc
        self.barrier()
        from contextlib import ExitStack
        with ExitStack() as es:
            sems = {}
            for e in ENGINES:
                sems[e] = es.enter_context(nc.semaphore(f"s_{e}"))
            for k in range(NDMA):
                sems[("dma", k)] = es.enter_context(nc.semaphore(f"s_dma{k}"))
            block = es.enter_context(nc.Block())
            engmap = {"pe": block.tensor, "act": block.scalar, "dve": block.vector,
                      "pool": block.gpsimd, "sp": block.sync}
            for e in ENGINES:
                oplist = self.ops[e]

                def body(engine, oplist=oplist):
                    for waits, fn, inc in oplist:
                        for key, val in waits:
                            engine.wait_ge(sems[key], val)
                            self.nwaits += 1
                        if fn is not None:
                            ins = fn(engine)
                            ins.then_inc(sems[inc[0]], inc[1])
                engmap[e](body)
from concourse.bass_utils import run_bass_kernel_spmd

T = 2048
D = 1024
NT = 16
EPS = 1e-6
NEG = -30000.0


def DR(ap):
    return V(ap, "dram", 0, 0)


class K:
    def __init__(self, S):
        self.S = S

    def mm(self, out, lhsT, rhs, start=True, stop=True):
        self.S.op("pe", lambda e: e.matmul(out.ap, lhsT=lhsT.ap, rhs=rhs.ap, start=start, stop=stop),
                  reads=[lhsT, rhs], writes=[out])

    def tr(self, out, in_, ident):
        self.S.op("pe", lambda e: e.transpose(out=out.ap, in_=in_.ap, identity=ident.ap),
                  reads=[in_, ident], writes=[out])

    def act(self, out, in_, func, bias=None, scale=None, accum=None):
        kw = {}
        rd = [in_]
        if bias is not None:
            kw["bias"] = bias.ap if isinstance(bias, V) else bias
            if isinstance(bias, V):
                rd.append(bias)
        if scale is not None:
            kw["scale"] = scale.ap if isinstance(scale, V) else scale
            if isinstance(scale, V):
                rd.append(scale)
        wr = [out]
        if accum is not None:
            kw["accum_out"] = accum.ap
            wr.append(accum)
        self.S.op("act", lambda e: e.activation(out=out.ap, in_=in_.ap, func=func, **kw), reads=rd, writes=wr)

    def ts(self, eng, out, in0, s1, op0, s2=None, op1=None, accum=None):
        rd = [in0] + [s for s in (s1, s2) if isinstance(s, V)]
        a1 = s1.ap if isinstance(s1, V) else s1
        a2 = s2.ap if isinstance(s2, V) else s2
        kw = {}
        wr = [out]
        if op1 is not None:
            kw["op1"] = op1
        if accum is not None:
            kw["accum_out"] = accum.ap
            wr.append(accum)
        self.S.op(eng, lambda e: e.tensor_scalar(out=out.ap, in0=in0.ap, scalar1=a1, scalar2=a2, op0=op0, **kw),
                  reads=rd, writes=wr)

    def tt(self, eng, out, in0, in1, op):
        self.S.op(eng, lambda e: e.tensor_tensor(out=out.ap, in0=in0.ap, in1=in1.ap, op=op),
                  reads=[in0, in1], writes=[out])

    def stt(self, out, in0, scalar, in1, op0, op1):
        rd = [in0, in1] + ([scalar] if isinstance(scalar, V) else [])
        sc = scalar.ap if isinstance(scalar, V) else scalar
        self.S.op("dve", lambda e: e.scalar_tensor_tensor(out=out.ap, in0=in0.ap, scalar=sc, in1=in1.ap, op0=op0, op1=op1),
                  reads=rd, writes=[out])

    def cp(self, eng, out, in_):
        if eng == "act":
            self.act(out, in_, AF.Copy)
        else:
            self.S.op(eng, lambda e: e.tensor_copy(out=out.ap, in_=in_.ap), reads=[in_], writes=[out])

    def red(self, out, in_, op, axis=AX.X):
        self.S.op("dve", lambda e: e.tensor_reduce(out=out.ap, in_=in_.ap, axis=axis, op=op), reads=[in_], writes=[out])

    def recip(self, out, in_):
        self.S.op("dve", lambda e: e.reciprocal(out=out.ap, in_=in_.ap), reads=[in_], writes=[out])

    def memset(self, eng, out, val):
        self.S.op(eng, lambda e: e.memset(out.ap, val), writes=[out])

    def load(self, out, dram_ap, eng="sp"):
        self.S.dma(eng, lambda e: e.dma_start(out=out.ap, in_=dram_ap), writes=[out])

    def load_nc(self, out, dram_ap, eng="sp"):
        self.S.dma(eng, lambda e: e.dma_start(out=out.ap, in_=dram_ap, allow_slow_non_contiguous=True), writes=[out])

    def store(self, dram_ap, in_, eng="sp"):
        self.S.dma(eng, lambda e: e.dma_start(out=dram_ap, in_=in_.ap), reads=[in_], is_output=True)

NCF = 8
NCB = 4


def make_consts():
    i = np.arange(128)
    a, b = i[:, None], i[None, :]
    ident = (a == b).astype(np.float32)
    U = (a <= b).astype(np.float32)
    ones = np.ones((128, 128), np.float32)
    UT = np.where(b >= a, 0.0, NEG).astype(np.float32)
    LT = np.where(b <= a, 0.0, NEG).astype(np.float32)
    SEL = np.zeros((128, 128), np.float32)
    SEL[127, :] = 1.0
    SUP = (b > a).astype(np.float32)
    cf = np.stack([ident, U, ones, UT, LT, SEL, SUP, ones], axis=1)
    cbsrc = np.stack([ident, ones, UT, LT], axis=1)
    kk = np.arange(128)[:, None]
    qq = np.arange(512)[None, :]
    dm = np.stack([np.where((128 * j + kk) // 64 <= qq // 64, 0.0, NEG) for j in range(4)], axis=0).astype(np.float32)
    dm = np.ascontiguousarray(dm.transpose(1, 0, 2))
    inv_freq = (500000.0 ** (-np.arange(0, 16, 2, dtype=np.float32) / np.float32(16))).astype(np.float32)
    ang = (np.arange(T, dtype=np.float32)[:, None] * inv_freq[None, :]).astype(np.float32)
    cos, sin = np.cos(ang).astype(np.float32), np.sin(ang).astype(np.float32)
    cos16 = np.concatenate([cos.T, cos.T], axis=0)
    sin16 = np.concatenate([sin.T, sin.T], axis=0)
    rope = np.ascontiguousarray(np.stack([cos16, sin16], axis=1))
    cosf = np.ones((128, T), np.float32)
    sinf = np.zeros((128, T), np.float32)
    for base in (0, 64):
        cosf[base:base + 16] = cos16
        sinf[base:base + 16] = sin16
    rope2 = np.ascontiguousarray(np.stack([cosf, sinf], axis=1))
    P = np.zeros((64, 64), np.float32)
    for d in range(8):
        P[d, d + 8] = -1.0
        P[d + 8, d] = 1.0
    PT = np.ascontiguousarray(P.T)
    return {"c_f": cf, "c_bsrc": cbsrc, "c_dmask": dm, "c_rope": rope, "c_rope2": rope2, "c_pt": PT}


PARAM_SHAPES = {
    "norm_g": (4, 4, 1024), "mlp_w1": (4, 1024, 4096), "mlp_w2": (4, 4096, 1024),
    "gdn_w_in": (2, 1024, 4112), "gdn_conv": (2, 4, 3072), "gdn_a_log": (2, 8), "gdn_dt_bias": (2, 8),
    "gdn_norm_g": (2, 128), "gdn_w_out": (2, 1024, 1024),
    "mlstm_w_in": (1, 1024, 3088), "mlstm_gate_b": (1, 2, 8), "mlstm_norm_g": (1, 1024), "mlstm_w_out": (1, 1024, 1024),
    "diff_w_in": (1, 1024, 3072), "diff_lambda": (1, 4, 64), "diff_norm_g": (1, 128), "diff_w_out": (1, 1024, 1024),
}
CONST_SHAPES = {"c_f": (128, NCF, 128), "c_bsrc": (128, NCB, 128), "c_dmask": (128, 4, 512),
                "c_rope": (16, 2, T), "c_rope2": (128, 2, T), "c_pt": (64, 64)}


class Ctx:
    pass


def build(plan):
    nc = bass.Bass("TRN2", target_bir_lowering=False)
    dr = {}
    dr["x"] = nc.dram_tensor("x", [T, D], F32, kind="ExternalInput").ap()
    for name, shp in list(CONST_SHAPES.items()):
        dr[name] = nc.dram_tensor(name, list(shp), F32, kind="ExternalInput").ap()
    needed = []

    def P(name, j):
        key = f"{name}_{j}"
        if key not in dr:
            dr[key] = nc.dram_tensor(key, list(PARAM_SHAPES[name][1:]), F32, kind="ExternalInput").ap()
            needed.append((key, name, j))
        return dr[key]
    out = nc.dram_tensor("out", [T, D], F32, kind="ExternalOutput").ap()
    S = Sched(nc)
    k = K(S)
    c = Ctx()
    c.S, c.k, c.dr, c.nc, c.P = S, k, dr, nc, P
    c.x = S.sb([NT, D], F32)
    c.cf = S.sb([NCF, 128], F32)
    c.cb = S.sb([NCB, 128], BF)
    c.gB = [S.sb([D], F32), S.sb([D], F32)]
    c.ss = S.sb([NT], F32)
    c.std = S.sb([NT], F32)
    c.rstd = S.sb([NT], F32)
    c.junk = S.sb([D], BF)
    c.hn = [S.sb([D], BF), S.sb([D], BF)]
    c.ident = c.cf[:, 0, :]
    c.U = c.cf[:, 1, :]
    c.ones = c.cf[:, 2, :]
    c.UTm = c.cf[:, 3, :]
    c.LTm = c.cf[:, 4, :]
    c.SEL = c.cf[:, 5, :]
    c.SUP = c.cf[:, 6, :]
    c.identb = c.cb[:, 0, :]
    c.onesb = c.cb[:, 1, :]
    c.UTb = c.cb[:, 2, :]
    c.LTb = c.cb[:, 3, :]
    c.hn_i = 0
    c.epsc = S.sb([4], F32)
    k.memset("dve", c.epsc[:, 0:1], EPS)
    k.memset("dve", c.epsc[:, 1:2], 1.0)
    k.memset("dve", c.epsc[:, 2:3], float(-0.5 * np.log(128.0)))
    k.memset("dve", c.epsc[:, 3:4], 0.0)
    k.load(c.cf.all(), dr["c_f"])
    k.load(c.cb.all(), dr["c_bsrc"], eng="pool")
    for i in range(NT):
        k.load(c.x[:, i, :], dr["x"][i * 128:(i + 1) * 128, :])
    c.base_top = S.sb_top
    for kind, i in plan:
        S.sb_top = c.base_top
        if kind == "mlp":
            mlp_layer(c, i)
        elif kind == "mix":
            m = i % 3
            if m == 0:
                gdn_layer(c, i)
            elif m == 1:
                mlstm_layer(c, i)
            else:
                diff_layer(c, i)
        S.barrier()
    for i in range(NT):
        k.store(out[i * 128:(i + 1) * 128, :], c.x[:, i, :])
    S.emit()
    nc.needed_params = needed
    return nc


def bcast_row(ap1d, n):
    return ap1d.partition_broadcast(128)


def prenorm(c, tiles, gB, hT, pT):
    k = c.k
    for t in tiles:
        k.act(c.junk.all(), c.x[:, t, :], AF.Square, accum=c.ss[:, t:t + 1])
    t0, t1 = tiles[0], tiles[-1] + 1
    k.act(c.std[:, t0:t1], c.ss[:, t0:t1], AF.Ln, scale=1.0 / D, bias=c.epsc[:, 0:1])
    k.act(c.rstd[:, t0:t1], c.std[:, t0:t1], AF.Exp, scale=-0.5)
    for j, t in enumerate(tiles):
        hn = c.hn[c.hn_i % 2]
        c.hn_i += 1
        k.stt(hn.all(), c.x[:, t, :], c.rstd[:, t:t + 1], gB.all(), ALU.mult, ALU.mult)
        for ch in range(8):
            k.tr(pT[:, ch, :], hn[:, ch * 128:(ch + 1) * 128], c.identb)
        k.cp("act" if j % 2 == 0 else "dve", hT[:, :, j * 128:(j + 1) * 128], pT.all())


def postnorm_add(c, tiles, src_of, gB, scratch_of=None):
    k = c.k
    for t in tiles:
        k.act(c.junk.all(), src_of(t), AF.Square, accum=c.ss[:, t:t + 1])
    t0, t1 = tiles[0], tiles[-1] + 1
    k.act(c.std[:, t0:t1], c.ss[:, t0:t1], AF.Ln, scale=1.0 / D, bias=c.epsc[:, 0:1])
    k.act(c.rstd[:, t0:t1], c.std[:, t0:t1], AF.Exp, scale=-0.5)
    for t in tiles:
        s = src_of(t)
        k.stt(s, s, c.rstd[:, t:t + 1], gB.all(), ALU.mult, ALU.mult)
        k.tt("pool", c.x[:, t, :], c.x[:, t, :], s, ALU.add)


def mlp_layer(c, li):
    S, k, dr = c.S, c.k, c.dr
    k.load(c.gB[0].all(), bcast_row(c.P("norm_g", li)[2], D))
    k.load(c.gB[1].all(), bcast_row(c.P("norm_g", li)[3], D))
    hTm = S.sb([8, 1024], BF)
    acc = S.sb([8, D], F32)
    hid = [S.sb([4, 1024], BF), S.sb([4, 1024], BF)]
    w1b = [S.sb([8, 512], BF), S.sb([8, 512], BF)]
    w2b = [S.sb([4, D], BF), S.sb([4, D], BF)]
    sq = [S.sb([512], F32), S.sb([512], F32)]
    pT = S.ps(0, [8, 128], BF)
    ph = [S.ps(2048, [512], F32), S.ps(4096, [512], F32)]
    po = [S.ps(6144, [512], F32), S.ps(8192, [512], F32)]
    w1 = c.P("mlp_w1", li)
    w2 = c.P("mlp_w2", li)
    nph = npo = 0
    for half in range(2):
        tiles = list(range(8 * half, 8 * half + 8))
        prenorm(c, tiles, c.gB[0], hTm, pT)
        for fb in range(8):
            b = fb % 2
            k.load(w1b[b].all(), w1[:, fb * 512:(fb + 1) * 512].rearrange("(c p) n -> p c n", p=128), eng="pool")
            k.load(w2b[b].all(), w2[fb * 512:(fb + 1) * 512, :].rearrange("(c p) n -> p c n", p=128), eng="pool")
            for fc in range(4):
                for tb in range(2):
                    p = ph[nph % 2]
                    s = sq[nph % 2]
                    nph += 1
                    for kc in range(8):
                        k.mm(p.all(), w1b[b][:, kc, fc * 128:(fc + 1) * 128], hTm[:, kc, tb * 512:(tb + 1) * 512],
                             start=(kc == 0), stop=(kc == 7))
                    k.act(s.all(), p.all(), AF.Square)
                    k.stt(hid[b][:, fc, tb * 512:(tb + 1) * 512], p.all(), 0.0, s.all(), ALU.is_gt, ALU.mult)
            for tt in range(8):
                for ch in range(2):
                    p = po[npo % 2]
                    npo += 1
                    for fc in range(4):
                        k.mm(p.all(), hid[b][:, fc, tt * 128:(tt + 1) * 128], w2b[b][:, fc, ch * 512:(ch + 1) * 512],
                             start=(fc == 0), stop=(fc == 3))
                    a = acc[:, tt, ch * 512:(ch + 1) * 512]
                    if fb == 0:
                        k.cp("act", a, p.all())
                    else:
                        k.tt("dve", a, p.all(), a, ALU.add)
        postnorm_add(c, tiles, lambda t: acc[:, t - 8 * half, :], c.gB[1])


def silu_parts(c, x_sb, tmp_a, tmp_b):
    k = c.k
    k.act(tmp_a, x_sb, AF.Exp, scale=-1.0)
    k.act(tmp_a, tmp_a, AF.Ln, bias=c.epsc[:, 1:2])
    k.act(tmp_b, tmp_a, AF.Exp, scale=-1.0)
    return tmp_b


def outproj_post(c, oTb, wo, msb, pbanks, tiles, gB):
    k = c.k
    n = 0
    for u in range(4):
        for ch in range(2):
            p = pbanks[n % 2]
            n += 1
            for kc in range(8):
                k.mm(p.all(), oTb[:, kc, u * 128:(u + 1) * 128], wo[:, kc, ch * 512:(ch + 1) * 512],
                     start=(kc == 0), stop=(kc == 7))
            k.cp("act", msb[:, u, ch * 512:(ch + 1) * 512], p.all())
    postnorm_add(c, tiles, lambda t: msb[:, t - tiles[0], :], gB)


import os
GSTOP = int(os.environ.get('GSTOP', '99'))


def gdn_layer(c, li):
    S, k = c.S, c.k
    j = li // 3
    W = c.P("gdn_w_in", j)
    CONV = c.P("gdn_conv", j)
    NG = c.P("norm_g", li)
    k.load(c.gB[0].all(), bcast_row(NG[0], D))
    k.load(c.gB[1].all(), bcast_row(NG[1], D))
    wo = S.sb([8, D], BF)
    k.load(wo.all(), c.P("gdn_w_out", j).rearrange("(c p) n -> p c n", p=128), eng="pool")
    wab = S.sb([8, 16], BF)
    k.load(wab.all(), W[:, 4096:4112].rearrange("(c p) n -> p c n", p=128), eng="pool")
    alB = S.sb([8], F32)
    dtB = S.sb([8], F32)
    gnB = S.sb([128], F32)
    k.load(alB.all(), bcast_row(c.P("gdn_a_log", j), 8))
    k.load(dtB.all(), bcast_row(c.P("gdn_dt_bias", j), 8))
    k.load(gnB.all(), bcast_row(c.P("gdn_norm_g", j), 128))
    negeA = S.sb([8], F32)
    k.act(negeA.all(), alB.all(), AF.Exp)
    k.ts("dve", negeA.all(), negeA.all(), -1.0, ALU.mult)
    cw = S.sb([24, 4], F32)
    Sst = S.sb([8, 128], F32)
    Sbf = S.sb([8, 128], BF)
    halo = S.sb([8, 3, 4], F32)
    k.memset("pool", Sst.all(), 0.0)
    k.memset("pool", Sbf.all(), 0.0)
    k.memset("pool", halo.all(), 0.0)
    hTb = S.sb([8, 512], BF)
    oTb = S.sb([8, 512], BF)
    wh = [S.sb([8, 512], BF), S.sb([8, 512], BF)]
    gab = S.sb([4, 16], F32)
    gtmp = S.sb([4, 8], F32)
    g_ = S.sb([4, 8], F32)
    beta = S.sb([4, 8], F32)
    nbeta = S.sb([4, 8], F32)
    gc = S.sb([4, 8], F32)
    gl = S.sb([4, 8], F32)
    egc = S.sb([4, 8], F32)
    ngc = S.sb([4, 8], F32)
    kdec = S.sb([4, 8], F32)
    egl = S.sb([4, 8], F32)
    ssg = S.sb([4], F32)
    rsg = S.sb([4], F32)
    off_blk = S.sb_top
    pre = [S.sb([516], F32) for _ in range(3)]
    cv = S.sb([512], F32)
    ta = S.sb([512], F32)
    tb = S.sb([512], F32)
    sqb = S.sb([512], BF)
    qkv = [S.sb([512], BF) for _ in range(3)]
    zs = S.sb([4, 128], F32)
    msb = S.sb_at(off_blk, [4, D], F32)
    assert off_blk + msb.nbytes <= S.sb_top
    off_unit = S.sb_top
    UB = []
    for u in range(4):
        b = Ctx()
        b.G1 = S.sb([128], F32)
        b.Dt = S.sb([128], F32)
        b.Dts = S.sb([128], F32)
        b.Ct = [S.sb([128], F32), S.sb([128], F32)]
        b.C = [S.sb([128], F32), S.sb([128], F32)]
        b.Rt = [S.sb([128], F32), S.sb([128], F32)]
        b.attnT = S.sb([128], BF)
        b.ke = S.sb([128], BF)
        b.kst = S.sb([128], BF)
        b.vtok = S.sb([128], BF)
        b.Rtb = S.sb([128], BF)
        b.nkcdT = S.sb([128], BF)
        b.vnew = S.sb([128], BF)
        b.o = S.sb([128], F32)
        b.otmp = S.sb([128], F32)
        b.og = S.sb([128], BF)
        base = (4 + u) * 2048
        b.s0 = S.ps(base, [128], F32)
        b.s12 = S.ps(base + 512, [256], F32)
        b.s1 = S.ps(base + 512, [128], F32)
        b.s2 = S.ps(base + 1024, [128], F32)
        b.s3 = S.ps(base + 1536, [128], F32)
        b.s3b = S.ps(base + 1536, [2, 128], BF)
        UB.append(b)
    convraw = S.sb_at(off_unit, [3072], F32, parts=4)
    pT = S.ps(0, [8, 128], BF)
    pin = [S.ps(0, [512], F32), S.ps(2048, [512], F32), S.ps(4096, [512], F32)]
    pss = S.ps(6144, [512], F32)
    pz = S.ps(6144, [4, 128], F32)
    pg = S.ps(6144, [4, 16], F32)
    pgc = S.ps(6144 + 256, [4, 8], F32)
    pgl = S.ps(6144 + 512, [4, 8], F32)
    pcw = S.ps(6144, [24, 4], F32)

    k.load(convraw.all(), CONV)
    for b_ in range(24):
        k.mm(pcw[:, b_, :], convraw[:, b_ * 128:(b_ + 1) * 128], c.cf[0:4, 0, 0:4])
    k.cp("dve", cw.all(), pcw.all())

    if GSTOP <= 1:
        return
    nwh = 0

    def load_head(hh, buf):
        for i in range(4):
            k.load(buf[:, :, i * 128:(i + 1) * 128],
                   W[:, i * 1024 + hh * 128:i * 1024 + (hh + 1) * 128].rearrange("(c p) n -> p c n", p=128), eng="pool")

    load_head(0, wh[0])
    for blk in range(4):
        tiles = list(range(blk * 4, blk * 4 + 4))
        prenorm(c, tiles, c.gB[0], hTb, pT)
        for u in range(4):
            for kc in range(8):
                k.mm(pg[:, u, :], hTb[:, kc, u * 128:(u + 1) * 128], wab[:, kc, :], start=(kc == 0), stop=(kc == 7))
        k.cp("dve", gab.all(), pg.all())
        for h in range(8):
            k.ts("dve", gtmp[:, :, h], gab[:, :, h], dtB[:, h:h + 1], ALU.add)
        k.act(gtmp.all(), gtmp.all(), AF.Exp)
        k.act(gtmp.all(), gtmp.all(), AF.Ln, bias=c.epsc[:, 1:2])
        for h in range(8):
            k.ts("dve", g_[:, :, h], gtmp[:, :, h], negeA[:, h:h + 1], ALU.mult)
        k.act(beta.all(), gab[:, :, 8:16], AF.Exp, scale=-1.0)
        k.act(beta.all(), beta.all(), AF.Ln, bias=c.epsc[:, 1:2])
        k.act(beta.all(), beta.all(), AF.Exp, scale=-1.0)
        k.ts("dve", nbeta.all(), beta.all(), -1.0, ALU.mult)
        for u in range(4):
            k.mm(pgc[:, u, :], c.U, g_[:, u, :])
            k.mm(pgl[:, u, :], c.ones, g_[:, u, :])
        k.cp("dve", gc.all(), pgc.all())
        k.cp("dve", gl.all(), pgl.all())
        k.act(egc.all(), gc.all(), AF.Exp)
        k.ts("dve", ngc.all(), gc.all(), -1.0, ALU.mult)
        k.tt("dve", kdec.all(), gl.all(), gc.all(), ALU.subtract)
        k.act(kdec.all(), kdec.all(), AF.Exp)
        k.act(egl.all(), gl.all(), AF.Exp)

        if GSTOP <= 2:
            return
        for h in range(8):
            wcur = wh[nwh % 2]
            nwh += 1
            nxt = blk * 8 + h + 1
            if nxt < 32:
                load_head(nxt % 8, wh[nwh % 2])
            for i in range(3):
                p = pin[i]
                for kc in range(8):
                    k.mm(p.all(), wcur[:, kc, i * 128:(i + 1) * 128], hTb[:, kc, :], start=(kc == 0), stop=(kc == 7))
                pr = pre[i]
                k.cp("pool", pr[:, 0:3], halo[:, h, i, 0:3])
                k.cp("act", pr[:, 3:515], p.all())
                bi = i * 8 + h
                k.ts("dve", cv.all(), pr[:, 3:515], cw[:, bi, 3:4], ALU.mult)
                for jj in (2, 1, 0):
                    k.stt(cv.all(), pr[:, jj:jj + 512], cw[:, bi, jj:jj + 1], cv.all(), ALU.mult, ALU.add)
                k.cp("pool", halo[:, h, i, 0:3], pr[:, 512:515])
                sg = silu_parts(c, cv.all(), ta.all(), tb.all())
                if i == 2:
                    k.tt("pool", qkv[2].all(), cv.all(), sg, ALU.mult)
                else:
                    k.tt("pool", cv.all(), cv.all(), sg, ALU.mult)
                    k.tt("pool", sqb.all(), cv.all(), cv.all(), ALU.mult)
                    k.mm(pss.all(), c.onesb, sqb.all())
                    k.act(ta.all(), pss.all(), AF.Ln, bias=c.epsc[:, 0:1])
                    k.act(tb.all(), ta.all(), AF.Exp, scale=-0.5, bias=(c.epsc[:, 2:3] if i == 0 else c.epsc[:, 3:4]))
                    k.tt("pool", qkv[i].all(), cv.all(), tb.all(), ALU.mult)
            qT, kT, vT = qkv
            if GSTOP <= 3:
                return
            for u in range(4):
                for kc in range(8):
                    k.mm(pz[:, u, :], hTb[:, kc, u * 128:(u + 1) * 128], wcur[:, kc, 384:512], start=(kc == 0), stop=(kc == 7))
            k.cp("act", zs.all(), pz.all())
            zflat = zs.all()
            ta4 = S.sb_at(ta.off, [4, 128], F32)
            tb4 = S.sb_at(tb.off, [4, 128], F32)
            sgz = silu_parts(c, zflat, ta4.all(), tb4.all())
            k.tt("pool", zs.all(), zs.all(), sgz, ALU.mult)

            stages = [[] for _ in range(4)]
            for u in range(4):
                b = UB[u]
                t = u
                cols = slice(u * 128, (u + 1) * 128)
                st = stages[u]

                def s_a(b=b, t=t, cols=cols):
                    k.tr(b.s3b[:, 0, :], kT[:, cols], c.identb)
                    k.tr(b.s3b[:, 1, :], vT[:, cols], c.identb)
                    k.ts("dve", b.ke.all(), b.s3b[:, 0, :], egc[:, t, h:h + 1], ALU.mult)
                    k.ts("dve", b.kst.all(), b.s3b[:, 0, :], kdec[:, t, h:h + 1], ALU.mult)
                    k.cp("act", b.vtok.all(), b.s3b[:, 1, :])
                    k.ts("pool", b.G1.all(), c.ones, g_[:, t, h:h + 1], ALU.mult)
                st.append(s_a)

                def s_b(b=b, t=t, cols=cols):
                    k.mm(b.s0.all(), b.G1.all(), c.U, start=True, stop=False)
                    k.mm(b.s0.all(), c.ident, c.UTm, start=False, stop=True)
                    k.mm(b.s12[:, 0:128], kT[:, cols], kT[:, cols])
                    k.mm(b.s12[:, 128:256], kT[:, cols], qT[:, cols])
                    k.act(b.Dt.all(), b.s0.all(), AF.Exp, bias=ngc[:, t, h:h + 1])
                st.append(s_b)

                def s_c(b=b, t=t):
                    k.tt("pool", b.Dts.all(), b.Dt.all(), c.SUP, ALU.mult)
                    k.tt("dve", b.attnT.all(), b.s12[:, 128:256], b.Dt.all(), ALU.mult)
                    k.stt(b.Ct[0].all(), b.s12[:, 0:128], nbeta[:, t, h:h + 1], b.Dts.all(), ALU.mult, ALU.mult)
                st.append(s_c)

                def s_d(b=b):
                    k.mm(b.s3.all(), b.Ct[0].all(), c.ident)
                    k.tt("pool", b.Rt[0].all(), b.Ct[0].all(), c.ident, ALU.add)
                    k.cp("act", b.C[0].all(), b.s3.all())
                st.append(s_d)
                for lv in range(1, 7):
                    def s_e(b=b, lv=lv):
                        po_, pn = (lv - 1) % 2, lv % 2
                        dbg = int(os.environ.get('GDBG', '0'))
                        if dbg in (0, 5, 6):
                            k.mm(b.s0.all(), b.Ct[po_].all(), b.C[po_].all())
                        if lv <= 5 and dbg in (0, 5):
                            k.mm(b.s1.all(), b.C[po_].all(), b.Ct[po_].all())
                        if dbg in (0, 6, 8):
                            k.cp("act", b.C[pn].all(), b.s0.all())
                        if lv <= 5 and dbg in (0, 7):
                            k.cp("dve", b.Ct[pn].all(), b.s1.all())
                        if dbg == 14:
                            k.cp("dve", b.Ct[pn].all(), b.Ct[po_].all())
                        if dbg == 15:
                            k.cp("dve", b.Ct[pn].all(), b.s0.all())
                        if dbg == 16:
                            k.cp("dve", b.o.all(), b.s1.all())
                        if lv <= 5 and dbg == 12:
                            k.ts("dve", b.Ct[pn].all(), b.s1.all(), 1.0, ALU.mult)
                        if lv <= 5 and dbg == 13:
                            k.cp("act", b.Ct[pn].all(), b.s1.all())
                        if dbg == 9:
                            k.mm(b.s0.all(), c.ident, c.U)
                            k.mm(b.s1.all(), c.U, c.ident)
                        if dbg == 10:
                            k.mm(b.s0.all(), b.Ct[po_].all(), c.U)
                        if dbg == 11:
                            k.mm(b.s0.all(), c.U, b.C[po_].all())
                    st.append(s_e)

                    def s_f(b=b, lv=lv):
                        po_, pn = (lv - 1) % 2, lv % 2
                        k.mm(b.s2.all(), b.C[pn].all(), b.Rt[po_].all())
                        k.tt("dve", b.Rt[pn].all(), b.s2.all(), b.Rt[po_].all(), ALU.add)
                    st.append(s_f)

                def s_g(b=b):
                    k.cp("pool", b.Rtb.all(), b.Rt[0].all())
                    k.mm(b.s0.all(), b.ke.all(), b.Rtb.all())
                    k.act(b.nkcdT.all(), b.s0.all(), AF.Copy, scale=-1.0)
                st.append(s_g)
            for si in range(min(len(stages[0]), int(os.environ.get('GSTAGE', '99')))):
                for u in range(4):
                    stages[u][si]()

            if GSTOP <= 4:
                return
            Sh = Sst[:, h, :]
            Shb = Sbf[:, h, :]
            for u in range(4):
                b = UB[u]
                t = u
                cols = slice(u * 128, (u + 1) * 128)
                k.mm(b.s1.all(), b.Rtb.all(), b.vtok.all(), start=True, stop=False)
                k.mm(b.s1.all(), b.nkcdT.all(), Shb, start=False, stop=True)
                k.act(b.vnew.all(), b.s1.all(), AF.Copy, scale=beta[:, t, h:h + 1])
                k.mm(b.s0.all(), qT[:, cols], Shb)
                k.mm(b.s3.all(), b.kst.all(), b.vnew.all())
                k.mm(b.s2.all(), b.attnT.all(), b.vnew.all())
                k.stt(Sh, Sh, egl[:, t, h:h + 1], b.s3.all(), ALU.mult, ALU.add)
                k.cp("pool", Shb, Sh)
                k.cp("act", b.otmp.all(), b.s2.all())
                k.stt(b.o.all(), b.s0.all(), egc[:, t, h:h + 1], b.otmp.all(), ALU.mult, ALU.add)
                k.act(b.otmp.all(), b.o.all(), AF.Square, accum=ssg[:, u:u + 1])
            k.act(rsg.all(), ssg.all(), AF.Ln, scale=1.0 / 128, bias=c.epsc[:, 0:1])
            k.act(rsg.all(), rsg.all(), AF.Exp, scale=-0.5)
            for u in range(4):
                b = UB[u]
                k.stt(b.o.all(), b.o.all(), rsg[:, u:u + 1], gnB.all(), ALU.mult, ALU.mult)
                k.tt("pool", b.og.all(), b.o.all(), zs[:, u, :], ALU.mult)
                k.tr(b.s3b[:, 0, :], b.og.all(), c.identb)
                k.cp("act", oTb[:, h, u * 128:(u + 1) * 128], b.s3b[:, 0, :])
        if GSTOP <= 5:
            return
        outproj_post(c, oTb, wo, msb, [pin[0], pin[1]], tiles, c.gB[1])


def sigmoid_parts(c, out, x, tmp, scale=1.0):
    k = c.k
    k.act(tmp, x, AF.Exp, scale=-scale)
    k.act(tmp, tmp, AF.Ln, bias=c.epsc[:, 1:2])
    k.act(out, tmp, AF.Exp, scale=-1.0)


def mlstm_layer(c, li):
    S, k = c.S, c.k
    j = li // 3
    W = c.P("mlstm_w_in", j)
    NG = c.P("norm_g", li)
    k.load(c.gB[0].all(), bcast_row(NG[0], D))
    k.load(c.gB[1].all(), bcast_row(NG[1], D))
    wo = S.sb([8, D], BF)
    k.load(wo.all(), c.P("mlstm_w_out", j).rearrange("(c p) n -> p c n", p=128), eng="pool")
    wif = S.sb([8, 16], BF)
    k.load(wif.all(), W[:, 3072:3088].rearrange("(c p) n -> p c n", p=128), eng="pool")
    gbB = S.sb([16], F32)
    k.load(gbB.all(), c.P("mlstm_gate_b", j).rearrange("a b -> (a b)").partition_broadcast(128))
    nrmB = S.sb([D], F32)
    k.load(nrmB.all(), bcast_row(c.P("mlstm_norm_g", j), D))
    Cn = S.sb([8, 132], F32)
    Cnb = S.sb([8, 132], BF)
    mst = S.sb([8], F32)
    k.memset("pool", Cn.all(), 0.0)
    k.memset("pool", Cnb.all(), 0.0)
    k.memset("pool", mst.all(), 0.0)
    hTb = S.sb([8, 512], BF)
    oTb = S.sb([8, 512], BF)
    wh = [S.sb([8, 384], BF), S.sb([8, 384], BF)]
    gab = S.sb([4, 16], F32)
    gt1 = S.sb([4, 16], F32)
    lf = S.sb([4, 8], F32)
    bc = S.sb([4, 8], F32)
    nbc = S.sb([4, 8], F32)
    r_ = S.sb([4, 8], F32)
    ssg = S.sb([4], F32)
    rsg = S.sb([4], F32)
    off_blk = S.sb_top
    qT = S.sb([512], BF)
    kT = S.sb([512], BF)
    ktok = S.sb([4, 64], F32)
    vaug = S.sb([4, 132], BF)
    og = S.sb([4, 128], F32)
    ogt = S.sb([4, 128], F32)
    S.sb_top = max(S.sb_top, off_blk + 4 * D * 4)
    msb = S.sb_at(off_blk, [4, D], F32)
    UB = []
    for u in range(4):
        b = Ctx()
        b.Rb = S.sb([128], F32)
        b.Pm = S.sb([128], F32)
        b.smat = S.sb([128], BF)
        b.sT = S.sb([128], BF)
        b.kw = S.sb([64], BF)
        b.nd = S.sb([132], F32)
        b.tmp = S.sb([132], F32)
        b.hc = S.sb([128], F32)
        b.hb = S.sb([128], BF)
        b.col = S.sb([16], F32)
        base = (4 + u) * 2048
        b.pD0 = S.ps(base, [128], F32)
        b.pQK = S.ps(base + 512, [128], F32)
        b.psT = S.ps(base, [128], BF)
        b.pQC = S.ps(base + 512, [129], F32)
        b.pSV = S.ps(base + 1032, [129], F32)
        b.pdC = S.ps(base, [129], F32)
        b.pmg = S.ps(base + 1552, [2], F32)
        b.pT2 = S.ps(base + 1600, [128], BF)
        UB.append(b)
    pT = S.ps(0, [8, 128], BF)
    pq = S.ps(0, [512], F32)
    pk = S.ps(2048, [512], F32)
    ptok = [S.ps(4096, [320], F32), S.ps(6144, [320], F32)]
    pg = S.ps(6144, [4, 16], F32)
    pbc = S.ps(6144 + 512, [4, 8], F32)
    nwh = 0

    def load_head(hh, buf):
        k.load(buf[:, :, 0:64], W[:, hh * 64:(hh + 1) * 64].rearrange("(c p) n -> p c n", p=128), eng="pool")
        k.load(buf[:, :, 64:128], W[:, 512 + hh * 64:512 + (hh + 1) * 64].rearrange("(c p) n -> p c n", p=128), eng="pool")
        k.load(buf[:, :, 128:256], W[:, 1024 + hh * 128:1024 + (hh + 1) * 128].rearrange("(c p) n -> p c n", p=128), eng="pool")
        k.load(buf[:, :, 256:384], W[:, 2048 + hh * 128:2048 + (hh + 1) * 128].rearrange("(c p) n -> p c n", p=128), eng="pool")

    load_head(0, wh[0])
    for blk in range(4):
        tiles = list(range(blk * 4, blk * 4 + 4))
        prenorm(c, tiles, c.gB[0], hTb, pT)
        for u in range(4):
            for kc in range(8):
                k.mm(pg[:, u, :], hTb[:, kc, u * 128:(u + 1) * 128], wif[:, kc, :], start=(kc == 0), stop=(kc == 7))
        for u in range(4):
            k.tt("dve", gab[:, u, :], pg[:, u, :], gbB.all(), ALU.add)
        sigmoid_parts(c, gab.all(), gab.all(), gt1.all(), scale=2.0 / 15.0)
        k.ts("dve", gab.all(), gab.all(), 30.0, ALU.mult, -15.0, ALU.add)
        k.act(lf.all(), gab[:, :, 8:16], AF.Exp, scale=-1.0)
        k.act(lf.all(), lf.all(), AF.Ln, bias=c.epsc[:, 1:2])
        k.ts("dve", lf.all(), lf.all(), -1.0, ALU.mult)
        for u in range(4):
            k.mm(pbc[:, u, :], c.U, lf[:, u, :])
        k.cp("dve", bc.all(), pbc.all())
        k.ts("dve", nbc.all(), bc.all(), -1.0, ALU.mult)
        k.tt("dve", r_.all(), gab[:, :, 0:8], bc.all(), ALU.subtract)

        for h in range(8):
            wcur = wh[nwh % 2]
            nwh += 1
            nxt = blk * 8 + h + 1
            if nxt < 32:
                load_head(nxt % 8, wh[nwh % 2])
            for kc in range(8):
                k.mm(pq[0:64, :], wcur[:, kc, 0:64], hTb[:, kc, :], start=(kc == 0), stop=(kc == 7))
            k.act(qT[0:64, :], pq[0:64, :], AF.Copy, scale=0.125)
            for kc in range(8):
                k.mm(pk[0:64, :], wcur[:, kc, 64:128], hTb[:, kc, :], start=(kc == 0), stop=(kc == 7))
            k.cp("dve", kT[0:64, :], pk[0:64, :])
            for u in range(4):
                p = ptok[u % 2]
                for kc in range(8):
                    k.mm(p.all(), hTb[:, kc, u * 128:(u + 1) * 128], wcur[:, kc, 64:384], start=(kc == 0), stop=(kc == 7))
                k.cp("act", ktok[:, u, :], p[:, 0:64])
                k.cp("dve", vaug[:, u, 0:128], p[:, 64:192])
                k.cp("act", og[:, u, :], p[:, 192:320])
            k.memset("pool", vaug[:, :, 128:129], 1.0)
            sigmoid_parts(c, og.all(), og.all(), ogt.all())
            mcol = mst[:, h:h + 1]
            Ch = Cn[0:64, h, 0:129]
            Chb = Cnb[0:64, h, 0:129]
            for u in range(4):
                b = UB[u]
                cols = slice(u * 128, (u + 1) * 128)
                k.ts("pool", b.Rb.all(), c.ones, r_[:, u, h:h + 1], ALU.mult)
                k.mm(b.pD0.all(), b.Rb.all(), c.ident, start=True, stop=False)
                k.mm(b.pD0.all(), c.ident, c.LTm, start=False, stop=True)
                k.mm(b.pQK.all(), qT[0:64, cols], kT[0:64, cols])
                k.red(b.col[:, 0:1], b.pD0.all(), ALU.max)
            for u in range(4):
                b = UB[u]
                cols = slice(u * 128, (u + 1) * 128)
                mx, mm_, nmm, wint, emo, stat, den = (b.col[:, i:i + 1] for i in range(7))
                k.tt("dve", mm_, mx, mcol, ALU.max)
                k.ts("dve", nmm, mm_, -1.0, ALU.mult)
                k.act(b.Pm.all(), b.pD0.all(), AF.Exp, bias=nmm)
                k.tt("dve", b.smat.all(), b.pQK.all(), b.Pm.all(), ALU.mult)
                k.act(wint, nmm, AF.Exp, bias=mcol)
                k.act(emo, bc[:, u, h:h + 1], AF.Exp, scale=-1.0, bias=nmm)
                k.tr(b.psT.all(), b.smat.all(), c.identb)
                k.cp("act", b.sT.all(), b.psT.all())
                k.mm(b.pQC[:, 0:129], qT[0:64, cols], Chb)
                k.mm(b.pSV[:, 0:129], b.sT.all(), vaug[:, u, 0:129])
                k.cp("act", b.tmp[:, 0:129], b.pSV[:, 0:129])
                k.stt(b.nd[:, 0:129], b.pQC[:, 0:129], wint, b.tmp[:, 0:129], ALU.mult, ALU.add)
                k.ts("dve", den, b.nd[:, 128:129], -1.0, ALU.mult)
                k.tt("dve", den, den, b.nd[:, 128:129], ALU.max)
                k.tt("dve", den, den, emo, ALU.max)
                k.recip(den, den)
                k.ts("dve", b.hc.all(), b.nd[:, 0:128], den, ALU.mult)
                k.act(b.tmp[:, 0:128], b.hc.all(), AF.Square, accum=ssg[:, u:u + 1])
                k.tt("dve", stat, bc[:, u, h:h + 1], mm_, ALU.add)
                k.cp("dve", b.col[:, 7:8], bc[:, u, h:h + 1])
                k.mm(b.pmg[:, 0:1], c.SEL, stat)
                k.mm(b.pmg[:, 1:2], c.SEL, b.col[:, 7:8])
                mg = b.col[:, 8:10]
                k.cp("dve", mg, b.pmg.all())
                mnew, gl = b.col[:, 8:9], b.col[:, 9:10]
                gm, wold, kwf = b.col[:, 10:11], b.col[:, 11:12], b.col[:, 12:13]
                k.tt("dve", gm, gl, mnew, ALU.subtract)
                k.act(wold, gm, AF.Exp, bias=mcol)
                k.act(kwf, r_[:, u, h:h + 1], AF.Exp, bias=gm)
                k.ts("dve", b.kw.all(), ktok[:, u, :], kwf, ALU.mult)
                k.mm(b.pdC[0:64, 0:129], b.kw.all(), vaug[:, u, 0:129])
                k.stt(Ch, Ch, wold[0:64, :] if False else b.col[0:64, 11:12], b.pdC[0:64, 0:129], ALU.mult, ALU.add)
                k.cp("pool", Chb, Ch)
                k.cp("dve", mcol, mnew)
            k.act(rsg.all(), ssg.all(), AF.Ln, scale=1.0 / 128, bias=c.epsc[:, 0:1])
            k.act(rsg.all(), rsg.all(), AF.Exp, scale=-0.5)
            for u in range(4):
                b = UB[u]
                k.stt(b.hc.all(), b.hc.all(), rsg[:, u:u + 1], nrmB[:, h * 128:(h + 1) * 128], ALU.mult, ALU.mult)
                k.tt("pool", b.hb.all(), b.hc.all(), og[:, u, :], ALU.mult)
                k.tr(b.pT2.all(), b.hb.all(), c.identb)
                k.cp("act", oTb[:, h, u * 128:(u + 1) * 128], b.pT2.all())
        outproj_post(c, oTb, wo, msb, [pq, pk], tiles, c.gB[1])


def diff_layer(c, li):
    import math
    S, k = c.S, c.k
    j = li // 3
    lam_init = 0.8 - 0.6 * math.exp(-0.3 * li)
    W = c.P("diff_w_in", j)
    WO = c.P("diff_w_out", j)
    NG = c.P("norm_g", li)
    k.load(c.gB[0].all(), bcast_row(NG[0], D))
    k.load(c.gB[1].all(), bcast_row(NG[1], D))
    kTall = S.sb([8, T], BF)
    vall = S.sb([16, 8, 129], BF)
    hTb = S.sb([8, 512], BF)
    oTb = S.sb([8, 512], BF)
    woh = S.sb([8, 512], BF)
    wh = S.sb([8, 384], BF)
    dmask = S.sb([4, 512], BF)
    ptb = S.sb([128], BF)
    small = S.sb([64], F32)
    gnB = S.sb([128], F32)
    lpB = S.sb([256], F32)
    ltmp = S.sb([64], F32)
    msb = S.sb([4, D], F32)
    o0 = msb.off
    q_sb = S.sb_at(o0, [512], BF)
    k_sb = S.sb_at(o0 + 1024, [512], BF)
    qT = S.sb_at(o0 + 2048, [512], BF)
    t1 = S.sb_at(o0 + 3072, [512], F32)
    t2 = S.sb_at(o0 + 5120, [512], F32)
    cs = S.sb_at(o0 + 7168, [2, 512], F32)
    PTt = [S.sb_at(o0 + 11264, [512], BF), S.sb_at(o0 + 12288, [512], BF)]
    ob = S.sb_at(o0 + 13312, [128], F32)
    obt = S.sb_at(o0 + 13824, [128], F32)
    obb = S.sb_at(o0 + 14336, [128], BF)
    zer = S.sb([512], BF)
    pT = S.ps(0, [8, 128], BF)
    pA = S.ps(0, [512], F32)
    pB = S.ps(2048, [512], F32)
    pv = S.ps(2048, [128], F32)
    pT2 = S.ps(2048, [128], BF)
    pS = [S.ps(4096, [512], F32), S.ps(6144, [512], F32)]
    acc = {}
    for cc in range(2):
        for u in range(4):
            bank = 4 + cc * 2 + u // 2
            acc[(cc, u)] = S.ps(bank * 2048 + (u % 2) * 1024, [129], F32)
    accbank = [S.ps((4 + i) * 2048, [512], F32) for i in range(4)]

    k.load(dmask.all(), c.dr["c_dmask"], eng="pool")
    k.memset("pool", ptb.all(), 0.0)
    k.load(ptb[0:64, 0:64], c.dr["c_pt"], eng="pool")
    k.load(ptb[64:128, 64:128], c.dr["c_pt"], eng="pool")
    k.memset("pool", vall[:, :, :, 128:129], 1.0)
    k.memset("pool", zer.all(), 0.0)
    k.load(gnB.all(), bcast_row(c.P("diff_norm_g", j), 128))
    k.ts("dve", gnB.all(), gnB.all(), float(1.0 - lam_init), ALU.mult)
    k.load(lpB.all(), c.P("diff_lambda", j).rearrange("a b -> (a b)").partition_broadcast(128))
    lam = small[:, 0:1]
    k.tt("dve", ltmp.all(), lpB[:, 0:64], lpB[:, 64:128], ALU.mult)
    k.red(small[:, 1:2], ltmp.all(), ALU.add)
    k.tt("dve", ltmp.all(), lpB[:, 128:192], lpB[:, 192:256], ALU.mult)
    k.red(small[:, 2:3], ltmp.all(), ALU.add)
    k.act(small[:, 1:3], small[:, 1:3], AF.Exp)
    k.tt("dve", lam, small[:, 1:2], small[:, 2:3], ALU.subtract)
    k.ts("dve", lam, lam, float(lam_init), ALU.add)
    nps = 0

    for blk in range(4):
        tiles = list(range(blk * 4, blk * 4 + 4))
        prenorm(c, tiles, c.gB[0], hTb, pT)
        k.load(cs.all(), c.dr["c_rope2"][:, :, blk * 512:(blk + 1) * 512])
        for h in range(8):
            for i in range(3):
                k.load(wh[:, :, i * 128:(i + 1) * 128],
                       W[:, i * 1024 + h * 128:i * 1024 + (h + 1) * 128].rearrange("(c p) n -> p c n", p=128), eng="pool")
            for i, (dst_sb, scale) in enumerate(((q_sb, 0.125), (k_sb, 1.0))):
                for kc in range(8):
                    k.mm(pA.all(), wh[:, kc, i * 128:(i + 1) * 128], hTb[:, kc, :], start=(kc == 0), stop=(kc == 7))
                k.act(dst_sb.all(), pA.all(), AF.Copy, scale=scale)
                k.mm(pB.all(), ptb.all(), dst_sb.all())
                k.tt("pool", t1.all(), dst_sb.all(), cs[:, 0, :], ALU.mult)
                k.tt("dve", t2.all(), pB.all(), cs[:, 1, :], ALU.mult)
                dst = qT.all() if i == 0 else kTall[:, h, blk * 512:(blk + 1) * 512]
                k.tt("pool", dst, t1.all(), t2.all(), ALU.add)
            for u in range(4):
                for kc in range(8):
                    k.mm(pv.all(), hTb[:, kc, u * 128:(u + 1) * 128], wh[:, kc, 256:384], start=(kc == 0), stop=(kc == 7))
                k.cp("act", vall[:, blk * 4 + u, h, 0:128], pv.all())
            for i in range(4):
                k.mm(accbank[i].all(), zer[:, 0:128], zer.all(), start=True, stop=True)
            nk = 4 * blk + 4
            for cc in range(2):
                pr0 = cc * 64
                for jt in range(nk):
                    p = pS[nps % 2]
                    pt_ = PTt[nps % 2]
                    nps += 1
                    diag = jt - 4 * blk
                    k.mm(p.all(), kTall[pr0:pr0 + 64, h, jt * 128:(jt + 1) * 128], qT[pr0:pr0 + 64, :],
                         start=True, stop=(diag < 0))
                    if diag >= 0:
                        k.mm(p.all(), c.identb, dmask[:, diag, :], start=False, stop=True)
                    k.act(pt_.all(), p.all(), AF.Exp)
                    for u in range(4):
                        if diag > u:
                            continue
                        k.mm(acc[(cc, u)].all(), pt_[:, u * 128:(u + 1) * 128], vall[:, jt, h, 0:129],
                             start=False, stop=(jt == 4 * blk + u))
            for u in range(4):
                a0, a1 = acc[(0, u)], acc[(1, u)]
                r0, r1 = small[:, 16 + u:17 + u], small[:, 20 + u:21 + u]
                k.recip(r0, a0[:, 128:129])
                k.recip(r1, a1[:, 128:129])
                k.tt("dve", r1, r1, lam, ALU.mult)
                k.ts("dve", obt.all(), a1[:, 0:128], r1, ALU.mult)
                k.stt(ob.all(), a0[:, 0:128], r0, obt.all(), ALU.mult, ALU.subtract)
                k.act(obt.all(), ob.all(), AF.Square, accum=small[:, 8 + u:9 + u])
                k.act(small[:, 12 + u:13 + u], small[:, 8 + u:9 + u], AF.Ln, scale=1.0 / 128, bias=c.epsc[:, 0:1])
                k.act(small[:, 12 + u:13 + u], small[:, 12 + u:13 + u], AF.Exp, scale=-0.5)
                k.stt(obb.all(), ob.all(), small[:, 12 + u:13 + u], gnB.all(), ALU.mult, ALU.mult)
                k.tr(pT2.all(), obb.all(), c.identb)
                k.cp("act", oTb[:, h, u * 128:(u + 1) * 128], pT2.all())
        n = 0
        for ch in range(2):
            k.load(woh.all(), WO[:, ch * 512:(ch + 1) * 512].rearrange("(c p) n -> p c n", p=128), eng="pool")
            for u in range(4):
                p = [pA, pB][n % 2]
                n += 1
                for kc in range(8):
                    k.mm(p.all(), oTb[:, kc, u * 128:(u + 1) * 128], woh[:, kc, :], start=(kc == 0), stop=(kc == 7))
                k.cp("act", msb[:, u, ch * 512:(ch + 1) * 512], p.all())
        postnorm_add(c, tiles, lambda t: msb[:, t - tiles[0], :], c.gB[1])

FULL_PLAN = [(kind, i) for i in range(4) for kind in ("mix", "mlp")]
_CONSTS = None


def run_plan(inputs, plan, x_override=None, trace=False, ncores=8):
    global _CONSTS
    if _CONSTS is None:
        _CONSTS = make_consts()
    nc = build(plan)
    x = np.ascontiguousarray(np.asarray(inputs["x"] if x_override is None else x_override, dtype=np.float32))
    shared = {key: np.ascontiguousarray(np.asarray(inputs[name], dtype=np.float32)[j]) for key, name, j in nc.needed_params}
    shared.update(_CONSTS)
    in_maps = []
    for b in range(ncores):
        m = dict(shared)
        m["x"] = x[b]
        in_maps.append(m)
    res = run_bass_kernel_spmd(nc, in_maps, core_ids=list(range(ncores)), trace=trace)
    outp = np.stack([np.asarray(res.results[b]["out"], dtype=np.float32) for b in range(ncores)], axis=0)
    return outp, res


def kernel(**inputs):
    outp, _ = run_plan(inputs, FULL_PLAN)
    return outp
```

```python
import numpy as np
import concourse.bass as bass
import concourse.mybir as mybir

F32 = mybir.dt.float32
BF = mybir.dt.bfloat16
F32R = mybir.dt.float32r
AF = mybir.ActivationFunctionType
ALU = mybir.AluOpType
AX = mybir.AxisListType
DSZ = {F32: 4, BF: 2, F32R: 4}

SB_CELL = 256
PS_CELL = 2048
ENGINES = ["pe", "act", "dve", "pool", "sp"]
NDMA = 24


class V:
    __slots__ = ("ap", "space", "lo", "hi")

    def __init__(self, ap, space, lo, hi):
        self.ap, self.space, self.lo, self.hi = ap, space, lo, hi


class Tn:
    def __init__(self, full_ap, space, off, shape, dtype, parts=128):
        self.full, self.space, self.off = full_ap, space, off
        self.shape, self.dtype, self.parts = tuple(shape), dtype, parts
        self.esz = DSZ[dtype]
        st, s = [], 1
        for d in reversed(self.shape):
            st.append(s)
            s *= d
        self.strides = tuple(reversed(st))
        self.nbytes = s * self.esz

    def __getitem__(self, idx):
        if not isinstance(idx, tuple):
            idx = (idx,)
        assert len(idx) == len(self.shape) + 1, (idx, self.shape)
        lo = hi = 0
        for k, (i, d, stn) in enumerate(zip(idx[1:], self.shape, self.strides)):
            if isinstance(i, slice):
                a = 0 if i.start is None else i.start
                b = d if i.stop is None else i.stop
                assert 0 <= a < b <= d, (idx, self.shape)
            else:
                a, b = i, i + 1
                assert 0 <= a < d
            lo += a * stn
            hi += (b - 1) * stn
        hi += 1
        return V(self.full[idx], self.space, self.off + lo * self.esz, self.off + hi * self.esz)

    def all(self):
        return self[(slice(None),) * (len(self.shape) + 1)]


class Sched:
    def __init__(self, nc, sb_bytes=212000, sbr_bytes=0):
        self.nc = nc
        self.sb_words = sb_bytes // 4
        self.arena = nc.alloc_sbuf_tensor("arena", [128, self.sb_words], F32)
        self.sbr_words = sbr_bytes // 4
        self.arena_r = nc.alloc_sbuf_tensor("arena_r", [128, self.sbr_words], F32R) if sbr_bytes else None
        self.sbr_top = 0
        self.psum = nc.alloc_psum_tensor("psum", [128, 4096], F32)
        self.sb_top = 0
        self.ops = {e: [] for e in ENGINES}
        self.count = {e: 0 for e in ENGINES}
        self.cells = {}
        self.seen = {e: {} for e in ENGINES}
        self.dma_cnt = [0] * NDMA
        self.dma_rr = 0
        self.out_tokens = []
        self.nwaits = 0
        self.q_hist = {e: [] for e in ENGINES}
        self.q_max = {"pool": 2, "sp": 8, "act": 4}

    def sb(self, shape, dtype, parts=128):
        n = int(np.prod(shape)) * DSZ[dtype]
        n = (n + 63) // 64 * 64
        off = self.sb_top
        self.sb_top += n
        assert self.sb_top <= self.sb_words * 4, f"SBUF overflow {self.sb_top}"
        return self.sb_at(off, shape, dtype, parts)

    def sbr(self, shape):
        n = int(np.prod(shape)) * 4
        off = self.sbr_top
        self.sbr_top += (n + 63) // 64 * 64
        assert self.sbr_top <= self.sbr_words * 4, "fp32r arena overflow"
        ap = self.arena_r[0:128, off // 4:(off + n) // 4]
        if len(shape) > 1:
            names = " ".join(f"d{i}" for i in range(len(shape)))
            kw = {f"d{i}": shape[i] for i in range(len(shape))}
            ap = ap.rearrange(f"p ({names}) -> p {names}", **kw)
        return Tn(ap, "sbr", off, shape, F32R, 128)

    def sb_at(self, off, shape, dtype, parts=128):
        n = int(np.prod(shape)) * DSZ[dtype]
        ap = self.arena[0:parts, off // 4:(off + n + 3) // 4]
        if dtype != F32:
            ap = ap.bitcast(dtype)
        if len(shape) > 1:
            names = " ".join(f"d{i}" for i in range(len(shape)))
            kw = {f"d{i}": shape[i] for i in range(len(shape))}
            ap = ap.rearrange(f"p ({names}) -> p {names}", **kw)
        return Tn(ap, "sb", off, shape, dtype, parts)

    def ps(self, off, shape, dtype=F32, parts=128):
        n = int(np.prod(shape)) * DSZ[dtype]
        assert off // 2048 == (off + n - 1) // 2048, "psum tensor crosses bank"
        ap = self.psum[0:parts, off // 4:(off + n + 3) // 4]
        if dtype != F32:
            ap = ap.bitcast(dtype)
        if len(shape) > 1:
            names = " ".join(f"d{i}" for i in range(len(shape)))
            kw = {f"d{i}": shape[i] for i in range(len(shape))}
            ap = ap.rearrange(f"p ({names}) -> p {names}", **kw)
        return Tn(ap, "ps", off, shape, dtype, parts)

    def _cells(self, v):
        c = PS_CELL if v.space == "ps" else SB_CELL
        return [(v.space, i) for i in range(v.lo // c, (v.hi - 1) // c + 1)]

    def _deps(self, eng, reads, writes, is_dma):
        deps = {}

        def need(tok, same_ok):
            if tok is None:
                return
            key, val, teng = tok
            if teng == eng and same_ok and not is_dma:
                return
            if deps.get(key, 0) < val:
                deps[key] = val

        for v in reads:
            if v.space == "dram":
                continue
            for c in self._cells(v):
                st = self.cells.get(c)
                if st:
                    need(st["w"], False)
                    if v.space == "ps":
                        for r in st["r"]:
                            need(r, True)
        for v in writes:
            if v.space == "dram":
                continue
            for c in self._cells(v):
                st = self.cells.get(c)
                if st:
                    need(st["w"], True)
                    for r in st["r"]:
                        need(r, True)
        return deps

    def _commit(self, tok, reads, writes):
        for v in reads:
            if v.space == "dram":
                continue
            for c in self._cells(v):
                st = self.cells.setdefault(c, {"w": None, "r": []})
                st["r"] = [r for r in st["r"] if r[0] != tok[0]] + [tok]
        for v in writes:
            if v.space == "dram":
                continue
            for c in self._cells(v):
                self.cells[c] = {"w": tok, "r": []}

    def _prune(self, eng, deps):
        out = []
        seen = self.seen[eng]
        for key, val in deps.items():
            if key == eng:
                pass
            if seen.get(key, 0) >= val:
                continue
            seen[key] = val
            out.append((key, val))
        return out

    def op(self, eng, fn, reads=(), writes=()):
        reads = [r for r in reads if r is not None]
        deps = self._deps(eng, reads, writes, False)
        waits = self._prune(eng, deps)
        self.count[eng] += 1
        tok = (eng, self.count[eng], eng)
        self.ops[eng].append((waits, fn, (eng, 1)))
        self._commit(tok, reads, writes)
        return tok

    def dma(self, eng, fn, reads=(), writes=(), is_output=False):
        k = self.dma_rr
        self.dma_rr = (self.dma_rr + 1) % NDMA
        deps = self._deps(eng, reads, writes, True)
        key = ("dma", k)
        if self.dma_cnt[k] > 0:
            deps[key] = max(deps.get(key, 0), self.dma_cnt[k])
        hist = self.q_hist[eng]
        qm = self.q_max.get(eng, 4)
        if len(hist) >= qm:
            pk, pv, _ = hist[-qm]
            deps[pk] = max(deps.get(pk, 0), pv)
        waits = self._prune(eng, deps)
        self.dma_cnt[k] += 16
        tok = (key, self.dma_cnt[k], "dmaq")
        self.ops[eng].append((waits, fn, (key, 16)))
        self._commit(tok, reads, writes)
        hist.append(tok)
        if is_output:
            self.out_tokens.append(tok)
        return tok

    def barrier(self):
        for e in ENGINES:
            deps = {}
            for x in ENGINES:
                if x != e and self.count[x] > 0:
                    deps[x] = self.count[x]
            for k in range(NDMA):
                if self.dma_cnt[k] > 0:
                    deps[("dma", k)] = self.dma_cnt[k]
            waits = self._prune(e, deps)
            if waits:
                self.ops[e].append((waits, None, None))

    def emit(self):
        nc = self.nc
        self.barrier()
        from contextlib import ExitStack
        with ExitStack() as es:
            sems = {}
            for e in ENGINES:
                sems[e] = es.enter_context(nc.semaphore(f"s_{e}"))
            for k in range(NDMA):
                sems[("dma", k)] = es.enter_context(nc.semaphore(f"s_dma{k}"))
            block = es.enter_context(nc.Block())
            engmap = {"pe": block.tensor, "act": block.scalar, "dve": block.vector,
                      "pool": block.gpsimd, "sp": block.sync}
            for e in ENGINES:
                oplist = self.ops[e]

                def body(engine, oplist=oplist):
                    for waits, fn, inc in oplist:
                        for key, val in waits:
                            engine.wait_ge(sems[key], val)
                            self.nwaits += 1
                        if fn is not None:
                            ins = fn(engine)
                            ins.then_inc(sems[inc[0]], inc[1])
                engmap[e](body)
from concourse.bass_utils import run_bass_kernel_spmd

T = 2048
D = 1024
NT = 16
EPS = 1e-6
NEG = -30000.0


def DR(ap):
    return V(ap, "dram", 0, 0)


def R32(v):
    return V(v.ap.bitcast(mybir.dt.float32r), v.space, v.lo, v.hi)


class K:
    def __init__(self, S):
        self.S = S

    def mm(self, out, lhsT, rhs, start=True, stop=True, r32=False):
        la, ra = lhsT.ap, rhs.ap
        if r32:
            la, ra = la.bitcast(mybir.dt.float32r), ra.bitcast(mybir.dt.float32r)
        self.S.op("pe", lambda e: e.matmul(out.ap, lhsT=la, rhs=ra, start=start, stop=stop),
                  reads=[lhsT, rhs], writes=[out])

    def tr(self, out, in_, ident):
        self.S.op("pe", lambda e: e.transpose(out=out.ap, in_=in_.ap, identity=ident.ap),
                  reads=[in_, ident], writes=[out])

    def act(self, out, in_, func, bias=None, scale=None, accum=None):
        kw = {}
        rd = [in_]
        if bias is not None:
            kw["bias"] = bias.ap if isinstance(bias, V) else bias
            if isinstance(bias, V):
                rd.append(bias)
        if scale is not None:
            kw["scale"] = scale.ap if isinstance(scale, V) else scale
            if isinstance(scale, V):
                rd.append(scale)
        wr = [out]
        if accum is not None:
            kw["accum_out"] = accum.ap
            wr.append(accum)
        self.S.op("act", lambda e: e.activation(out=out.ap, in_=in_.ap, func=func, **kw), reads=rd, writes=wr)

    def ts(self, eng, out, in0, s1, op0, s2=None, op1=None, accum=None):
        rd = [in0] + [s for s in (s1, s2) if isinstance(s, V)]
        a1 = s1.ap if isinstance(s1, V) else s1
        a2 = s2.ap if isinstance(s2, V) else s2
        kw = {}
        wr = [out]
        if op1 is not None:
            kw["op1"] = op1
        if accum is not None:
            kw["accum_out"] = accum.ap
            wr.append(accum)
        self.S.op(eng, lambda e: e.tensor_scalar(out=out.ap, in0=in0.ap, scalar1=a1, scalar2=a2, op0=op0, **kw),
                  reads=rd, writes=wr)

    def tt(self, eng, out, in0, in1, op):
        self.S.op(eng, lambda e: e.tensor_tensor(out=out.ap, in0=in0.ap, in1=in1.ap, op=op),
                  reads=[in0, in1], writes=[out])

    def stt(self, out, in0, scalar, in1, op0, op1):
        rd = [in0, in1] + ([scalar] if isinstance(scalar, V) else [])
        sc = scalar.ap if isinstance(scalar, V) else scalar
        self.S.op("dve", lambda e: e.scalar_tensor_tensor(out=out.ap, in0=in0.ap, scalar=sc, in1=in1.ap, op0=op0, op1=op1),
                  reads=rd, writes=[out])

    def cp(self, eng, out, in_):
        if eng == "act":
            self.act(out, in_, AF.Copy)
        else:
            self.S.op(eng, lambda e: e.tensor_copy(out=out.ap, in_=in_.ap), reads=[in_], writes=[out])

    def red(self, out, in_, op, axis=AX.X):
        self.S.op("dve", lambda e: e.tensor_reduce(out=out.ap, in_=in_.ap, axis=axis, op=op), reads=[in_], writes=[out])

    def recip(self, out, in_):
        self.S.op("dve", lambda e: e.reciprocal(out=out.ap, in_=in_.ap), reads=[in_], writes=[out])

    def memset(self, eng, out, val):
        self.S.op(eng, lambda e: e.memset(out.ap, val), writes=[out])

    def load(self, out, dram_ap, eng="sp"):
        self.S.dma(eng, lambda e: e.dma_start(out=out.ap, in_=dram_ap), writes=[out])

    def load_nc(self, out, dram_ap, eng="sp"):
        self.S.dma(eng, lambda e: e.dma_start(out=out.ap, in_=dram_ap, allow_slow_non_contiguous=True), writes=[out])

    def store(self, dram_ap, in_, eng="sp"):
        self.S.dma(eng, lambda e: e.dma_start(out=dram_ap, in_=in_.ap), reads=[in_], is_output=True)

NCF = 8
NCB = 4


def make_consts():
    i = np.arange(128)
    a, b = i[:, None], i[None, :]
    ident = (a == b).astype(np.float32)
    U = (a <= b).astype(np.float32)
    ones = np.ones((128, 128), np.float32)
    UT = np.where(b >= a, 0.0, NEG).astype(np.float32)
    LT = np.where(b <= a, 0.0, NEG).astype(np.float32)
    SEL = np.zeros((128, 128), np.float32)
    SEL[127, :] = 1.0
    SUP = (b > a).astype(np.float32)
    cf = np.stack([ident, U, ones, UT, LT, SEL, SUP, ones], axis=1)
    cbsrc = np.stack([ident, ones, UT, LT], axis=1)
    kk = np.arange(128)[:, None]
    qq = np.arange(512)[None, :]
    dm = np.stack([np.where((128 * j + kk) // 64 <= qq // 64, 0.0, NEG) for j in range(4)], axis=0).astype(np.float32)
    dm = np.ascontiguousarray(dm.transpose(1, 0, 2))
    inv_freq = (500000.0 ** (-np.arange(0, 16, 2, dtype=np.float32) / np.float32(16))).astype(np.float32)
    ang = (np.arange(T, dtype=np.float32)[:, None] * inv_freq[None, :]).astype(np.float32)
    cos, sin = np.cos(ang).astype(np.float32), np.sin(ang).astype(np.float32)
    cos16 = np.concatenate([cos.T, cos.T], axis=0)
    sin16 = np.concatenate([sin.T, sin.T], axis=0)
    rope = np.ascontiguousarray(np.stack([cos16, sin16], axis=1))
    cosf = np.ones((128, T), np.float32)
    sinf = np.zeros((128, T), np.float32)
    for base in (0, 64):
        cosf[base:base + 16] = cos16
        sinf[base:base + 16] = sin16
    rope2 = np.ascontiguousarray(np.stack([cosf, sinf], axis=1))
    P = np.zeros((64, 64), np.float32)
    for d in range(8):
        P[d, d + 8] = -1.0
        P[d + 8, d] = 1.0
    PT = np.ascontiguousarray(P.T)
    return {"c_f": cf, "c_bsrc": cbsrc, "c_dmask": dm, "c_rope": rope, "c_rope2": rope2, "c_pt": PT}


PARAM_SHAPES = {
    "norm_g": (4, 4, 1024), "mlp_w1": (4, 1024, 4096), "mlp_w2": (4, 4096, 1024),
    "gdn_w_in": (2, 1024, 4112), "gdn_conv": (2, 4, 3072), "gdn_a_log": (2, 8), "gdn_dt_bias": (2, 8),
    "gdn_norm_g": (2, 128), "gdn_w_out": (2, 1024, 1024),
    "mlstm_w_in": (1, 1024, 3088), "mlstm_gate_b": (1, 2, 8), "mlstm_norm_g": (1, 1024), "mlstm_w_out": (1, 1024, 1024),
    "diff_w_in": (1, 1024, 3072), "diff_lambda": (1, 4, 64), "diff_norm_g": (1, 128), "diff_w_out": (1, 1024, 1024),
}
CONST_SHAPES = {"c_f": (128, NCF, 128), "c_bsrc": (128, NCB, 128), "c_dmask": (128, 4, 512),
                "c_rope": (16, 2, T), "c_rope2": (128, 2, T), "c_pt": (64, 64)}


class Ctx:
    pass


def build(plan):
    nc = bass.Bass("TRN2", target_bir_lowering=False)
    dr = {}
    dr["x"] = nc.dram_tensor("x", [T, D], F32, kind="ExternalInput").ap()
    for name, shp in list(CONST_SHAPES.items()):
        dr[name] = nc.dram_tensor(name, list(shp), F32, kind="ExternalInput").ap()
    needed = []

    def P(name, j):
        key = f"{name}_{j}"
        if key not in dr:
            dr[key] = nc.dram_tensor(key, list(PARAM_SHAPES[name][1:]), F32, kind="ExternalInput").ap()
            needed.append((key, name, j))
        return dr[key]
    out = nc.dram_tensor("out", [T, D], F32, kind="ExternalOutput").ap()
    S = Sched(nc)
    k = K(S)
    c = Ctx()
    c.S, c.k, c.dr, c.nc, c.P = S, k, dr, nc, P
    c.x = S.sb([NT, D], F32)
    c.cf = S.sb([NCF, 128], F32)
    c.cb = S.sb([NCB, 128], BF)
    c.gB = [S.sb([D], F32), S.sb([D], F32)]
    c.ss = S.sb([NT], F32)
    c.std = S.sb([NT], F32)
    c.rstd = S.sb([NT], F32)
    c.junk = S.sb([D], BF)
    _hn = S.sb([D], BF)
    c.hn = [_hn, _hn]
    c.ident = c.cf[:, 0, :]
    c.U = c.cf[:, 1, :]
    c.ones = c.cf[:, 2, :]
    c.UTm = c.cf[:, 3, :]
    c.LTm = c.cf[:, 4, :]
    c.SEL = c.cf[:, 5, :]
    c.SUP = c.cf[:, 6, :]
    c.identb = c.cb[:, 0, :]
    c.onesb = c.cb[:, 1, :]
    c.UTb = c.cb[:, 2, :]
    c.LTb = c.cb[:, 3, :]
    c.hn_i = 0
    c.epsc = S.sb([4], F32)
    k.memset("dve", c.epsc[:, 0:1], EPS)
    k.memset("dve", c.epsc[:, 1:2], 1.0)
    k.memset("dve", c.epsc[:, 2:3], float(-0.5 * np.log(128.0)))
    k.memset("dve", c.epsc[:, 3:4], 0.0)
    k.load(c.cf.all(), dr["c_f"])
    k.load(c.cb.all(), dr["c_bsrc"], eng="pool")
    for i in range(NT):
        k.load(c.x[:, i, :], dr["x"][i * 128:(i + 1) * 128, :])
    c.base_top = S.sb_top
    for kind, i in plan:
        S.sb_top = c.base_top
        S.sbr_top = 0
        if kind == "mlp":
            mlp_layer(c, i)
        elif kind == "mix":
            m = i % 3
            if m == 0:
                gdn_layer(c, i)
            elif m == 1:
                mlstm_layer(c, i)
            else:
                diff_layer(c, i)
        S.barrier()
    for i in range(NT):
        k.store(out[i * 128:(i + 1) * 128, :], c.x[:, i, :])
    S.emit()
    nc.needed_params = needed
    return nc


def bcast_row(ap1d, n):
    return ap1d.partition_broadcast(128)


def prenorm(c, tiles, gB, hT, pT):
    k = c.k
    for t in tiles:
        k.act(c.junk.all(), c.x[:, t, :], AF.Square, accum=c.ss[:, t:t + 1])
    t0, t1 = tiles[0], tiles[-1] + 1
    k.act(c.std[:, t0:t1], c.ss[:, t0:t1], AF.Ln, scale=1.0 / D, bias=c.epsc[:, 0:1])
    k.act(c.rstd[:, t0:t1], c.std[:, t0:t1], AF.Exp, scale=-0.5)
    for j, t in enumerate(tiles):
        hn = c.hn[c.hn_i % 2]
        c.hn_i += 1
        k.stt(hn.all(), c.x[:, t, :], c.rstd[:, t:t + 1], gB.all(), ALU.mult, ALU.mult)
        for ch in range(8):
            k.tr(pT[:, ch, :], hn[:, ch * 128:(ch + 1) * 128], c.identb)
        k.cp("act" if j % 2 == 0 else "dve", hT[:, :, j * 128:(j + 1) * 128], pT.all())


def postnorm_add(c, tiles, src_of, gB, scratch_of=None):
    k = c.k
    for t in tiles:
        k.act(c.junk.all(), src_of(t), AF.Square, accum=c.ss[:, t:t + 1])
    t0, t1 = tiles[0], tiles[-1] + 1
    k.act(c.std[:, t0:t1], c.ss[:, t0:t1], AF.Ln, scale=1.0 / D, bias=c.epsc[:, 0:1])
    k.act(c.rstd[:, t0:t1], c.std[:, t0:t1], AF.Exp, scale=-0.5)
    for t in tiles:
        s = src_of(t)
        k.stt(s, s, c.rstd[:, t:t + 1], gB.all(), ALU.mult, ALU.mult)
        k.tt("pool", c.x[:, t, :], c.x[:, t, :], s, ALU.add)


def mlp_layer(c, li):
    S, k, dr = c.S, c.k, c.dr
    k.load(c.gB[0].all(), bcast_row(c.P("norm_g", li)[2], D))
    k.load(c.gB[1].all(), bcast_row(c.P("norm_g", li)[3], D))
    hTm = S.sb([8, 1024], BF)
    acc = S.sb([8, D], F32)
    hid = [S.sb([4, 1024], BF), S.sb([4, 1024], BF)]
    w1b = [S.sb([8, 512], BF), S.sb([8, 512], BF)]
    w2b = [S.sb([4, D], BF), S.sb([4, D], BF)]
    sq = [S.sb([512], F32), S.sb([512], F32)]
    pT = S.ps(0, [8, 128], BF)
    ph = [S.ps(2048, [512], F32), S.ps(4096, [512], F32)]
    po = [S.ps(6144, [512], F32), S.ps(8192, [512], F32)]
    w1 = c.P("mlp_w1", li)
    w2 = c.P("mlp_w2", li)
    nph = npo = 0
    for half in range(2):
        tiles = list(range(8 * half, 8 * half + 8))
        prenorm(c, tiles, c.gB[0], hTm, pT)
        for fb in range(8):
            b = fb % 2
            k.load(w1b[b].all(), w1[:, fb * 512:(fb + 1) * 512].rearrange("(c p) n -> p c n", p=128), eng="pool")
            k.load(w2b[b].all(), w2[fb * 512:(fb + 1) * 512, :].rearrange("(c p) n -> p c n", p=128), eng="pool")
            for fc in range(4):
                for tb in range(2):
                    p = ph[nph % 2]
                    s = sq[nph % 2]
                    nph += 1
                    for kc in range(8):
                        k.mm(p.all(), w1b[b][:, kc, fc * 128:(fc + 1) * 128], hTm[:, kc, tb * 512:(tb + 1) * 512],
                             start=(kc == 0), stop=(kc == 7))
                    k.act(s.all(), p.all(), AF.Square)
                    k.stt(hid[b][:, fc, tb * 512:(tb + 1) * 512], p.all(), 0.0, s.all(), ALU.is_gt, ALU.mult)
            for tt in range(8):
                for ch in range(2):
                    p = po[npo % 2]
                    npo += 1
                    for fc in range(4):
                        k.mm(p.all(), hid[b][:, fc, tt * 128:(tt + 1) * 128], w2b[b][:, fc, ch * 512:(ch + 1) * 512],
                             start=(fc == 0), stop=(fc == 3))
                    a = acc[:, tt, ch * 512:(ch + 1) * 512]
                    if fb == 0:
                        k.cp("act", a, p.all())
                    else:
                        k.tt("dve", a, p.all(), a, ALU.add)
        postnorm_add(c, tiles, lambda t: acc[:, t - 8 * half, :], c.gB[1])


def silu_parts(c, x_sb, tmp_a, tmp_b):
    k = c.k
    k.act(tmp_a, x_sb, AF.Exp, scale=-1.0)
    k.act(tmp_a, tmp_a, AF.Ln, bias=c.epsc[:, 1:2])
    k.act(tmp_b, tmp_a, AF.Exp, scale=-1.0)
    return tmp_b


def outproj_post(c, oTb, wo, msb, pbanks, tiles, gB):
    k = c.k
    n = 0
    for u in range(4):
        for ch in range(2):
            p = pbanks[n % 2]
            n += 1
            for kc in range(8):
                k.mm(p.all(), oTb[:, kc, u * 128:(u + 1) * 128], wo[:, kc, ch * 512:(ch + 1) * 512],
                     start=(kc == 0), stop=(kc == 7))
            k.cp("act", msb[:, u, ch * 512:(ch + 1) * 512], p.all())
    postnorm_add(c, tiles, lambda t: msb[:, t - tiles[0], :], gB)


import os
GSTOP = int(os.environ.get('GSTOP', '99'))


def gdn_layer(c, li):
    S, k = c.S, c.k
    j = li // 3
    W = c.P("gdn_w_in", j)
    CONV = c.P("gdn_conv", j)
    NG = c.P("norm_g", li)
    k.load(c.gB[0].all(), bcast_row(NG[0], D))
    k.load(c.gB[1].all(), bcast_row(NG[1], D))
    woh = S.sb([8, 512], BF)
    WO = c.P("gdn_w_out", j)
    wab = S.sb([8, 16], BF)
    k.load(wab.all(), W[:, 4096:4112].rearrange("(c p) n -> p c n", p=128), eng="pool")
    alB = S.sb([8], F32)
    dtB = S.sb([8], F32)
    gnB = S.sb([128], F32)
    k.load(alB.all(), bcast_row(c.P("gdn_a_log", j), 8))
    k.load(dtB.all(), bcast_row(c.P("gdn_dt_bias", j), 8))
    k.load(gnB.all(), bcast_row(c.P("gdn_norm_g", j), 128))
    negeA = S.sb([8], F32)
    k.act(negeA.all(), alB.all(), AF.Exp)
    k.ts("dve", negeA.all(), negeA.all(), -1.0, ALU.mult)
    cw = S.sb([24, 4], F32)
    Sst = S.sb([8, 128], F32)
    Sbf = S.sb([8, 128], BF)
    halo = S.sb([8, 3, 4], F32)
    k.memset("pool", Sst.all(), 0.0)
    k.memset("pool", Sbf.all(), 0.0)
    k.memset("pool", halo.all(), 0.0)
    hTb = S.sb([8, 512], BF)
    oTb = S.sb([8, 512], BF)
    wh = [S.sb([8, 512], BF), S.sb([8, 512], BF)]
    gab = S.sb([4, 16], F32)
    gtmp = S.sb([4, 8], F32)
    g_ = S.sb([4, 8], F32)
    beta = S.sb([4, 8], F32)
    nbeta = S.sb([4, 8], F32)
    gc = S.sb([4, 8], F32)
    gl = S.sb([4, 8], F32)
    egc = S.sb([4, 8], F32)
    ngc = S.sb([4, 8], F32)
    kdec = S.sb([4, 8], F32)
    egl = S.sb([4, 8], F32)
    ssg = S.sb([4], F32)
    rsg = S.sb([4], F32)
    off_blk = S.sb_top
    pre = [S.sb([516], F32) for _ in range(3)]
    cvs = [S.sb([512], F32) for _ in range(3)]
    tas = [S.sb([512], F32) for _ in range(3)]
    tbs = [S.sb([512], F32) for _ in range(3)]
    sqbs = [S.sb([512], BF) for _ in range(2)]
    qkvs = [[S.sb([512], BF) for _ in range(3)] for _ in range(2)]
    zss = [S.sb([4, 128], F32) for _ in range(2)]
    msb = S.sb_at(off_blk, [4, D], F32)
    assert off_blk + msb.nbytes <= S.sb_top
    off_unit = S.sb_top
    UB = []
    for u in range(4):
        b = Ctx()
        b.G1 = S.sb([128], F32)
        b.Dt = S.sb([128], F32)
        b.Dts = S.sb([128], F32)
        b.X = [S.sb([384], F32), S.sb([384], F32)]
        b.attnT = S.sb([128], BF)
        b.ke = S.sb([128], BF)
        b.kst = S.sb([128], BF)
        b.vtok = S.sb([128], BF)
        b.Rtb = S.sb([128], BF)
        b.nkcdT = S.sb([128], BF)
        b.vnew = S.sb([128], BF)
        b.o = S.sb([128], F32)
        b.otmp = S.sb([128], F32)
        b.og = S.sb([128], BF)
        base = (4 + u) * 2048
        b.s012 = S.ps(base, [384], F32)
        b.s0 = S.ps(base, [128], F32)
        b.s12 = S.ps(base + 512, [256], F32)
        b.s1 = S.ps(base + 512, [128], F32)
        b.s2 = S.ps(base + 1024, [128], F32)
        b.s3 = S.ps(base + 1536, [128], F32)
        b.s3b = S.ps(base + 1536, [2, 128], BF)
        UB.append(b)
    convraw = S.sb_at(off_unit, [3072], F32, parts=4)
    pT = S.ps(0, [8, 128], BF)
    pin = [S.ps(0, [512], F32), S.ps(2048, [512], F32), S.ps(4096, [512], F32)]
    pss = S.ps(6144, [512], F32)
    pz = S.ps(6144, [4, 128], F32)
    pg = S.ps(6144, [4, 16], F32)
    pgc = S.ps(6144 + 256, [4, 8], F32)
    pgl = S.ps(6144 + 512, [4, 8], F32)
    pcw = S.ps(6144, [24, 4], F32)

    k.load(convraw.all(), CONV)
    for b_ in range(24):
        k.mm(pcw[:, b_, :], convraw[:, b_ * 128:(b_ + 1) * 128], c.cf[0:4, 0, 0:4])
    k.cp("dve", cw.all(), pcw.all())

    if GSTOP <= 1:
        return
    nwh = 0

    def load_head(hh, buf):
        for i in range(4):
            k.load(buf[:, :, i * 128:(i + 1) * 128],
                   W[:, i * 1024 + hh * 128:i * 1024 + (hh + 1) * 128].rearrange("(c p) n -> p c n", p=128), eng="pool")


    def merge_run(P, Q):
        n, m = len(P), len(Q)
        j = 0
        for i in range(n):
            P[i]()
            tgt = (i + 1) * m // n
            while j < tgt:
                Q[j]()
                j += 1
        while j < m:
            Q[j]()
            j += 1

    def inproj_steps(h, wcur, par):
        st = []
        qkv = qkvs[par]
        zs = zss[par]
        for i in range(3):
            p = pin[i]
            pr = pre[i]
            bi = i * 8 + h
            cvi, tai, tbi = cvs[i], tas[i], tbs[i]

            def f0(i=i, p=p):
                for kc in range(8):
                    k.mm(p.all(), wcur[:, kc, i * 128:(i + 1) * 128], hTb[:, kc, :], start=(kc == 0), stop=(kc == 7))
            st.append(f0)

            def f1(i=i, p=p, pr=pr):
                k.cp("pool", pr[:, 0:3], halo[:, h, i, 0:3])
                k.cp("act", pr[:, 3:515], p.all())
            st.append(f1)

            def f2(pr=pr, bi=bi, cvi=cvi):
                k.ts("dve", cvi.all(), pr[:, 3:515], cw[:, bi, 3:4], ALU.mult)
                for jj in (2, 1):
                    k.stt(cvi.all(), pr[:, jj:jj + 512], cw[:, bi, jj:jj + 1], cvi.all(), ALU.mult, ALU.add)
            st.append(f2)

            def f3(i=i, pr=pr, bi=bi, cvi=cvi):
                k.stt(cvi.all(), pr[:, 0:512], cw[:, bi, 0:1], cvi.all(), ALU.mult, ALU.add)
                k.cp("pool", halo[:, h, i, 0:3], pr[:, 512:515])
            st.append(f3)

            def f4(cvi=cvi, tai=tai):
                k.act(tai.all(), cvi.all(), AF.Exp, scale=-1.0)
                k.act(tai.all(), tai.all(), AF.Ln, bias=c.epsc[:, 1:2])
            st.append(f4)

            def f5(i=i, cvi=cvi, tai=tai, tbi=tbi):
                k.act(tbi.all(), tai.all(), AF.Exp, scale=-1.0)
                if i == 2:
                    k.tt("pool", qkv[2].all(), cvi.all(), tbi.all(), ALU.mult)
                else:
                    k.tt("pool", cvi.all(), cvi.all(), tbi.all(), ALU.mult)
                    k.tt("pool", sqbs[i].all(), cvi.all(), cvi.all(), ALU.mult)
            st.append(f5)
            if i < 2:
                def f6(i=i, cvi=cvi, tai=tai, tbi=tbi):
                    k.mm(pss.all(), c.onesb, sqbs[i].all())
                    k.act(tai.all(), pss.all(), AF.Ln, bias=c.epsc[:, 0:1])
                st.append(f6)

                def f7(i=i, cvi=cvi, tai=tai, tbi=tbi):
                    k.act(tbi.all(), tai.all(), AF.Exp, scale=-0.5, bias=(c.epsc[:, 2:3] if i == 0 else c.epsc[:, 3:4]))
                    k.tt("pool", qkv[i].all(), cvi.all(), tbi.all(), ALU.mult)
                st.append(f7)

        def fz0():
            for u in range(4):
                for kc in range(8):
                    k.mm(pz[:, u, :], hTb[:, kc, u * 128:(u + 1) * 128], wcur[:, kc, 384:512], start=(kc == 0), stop=(kc == 7))
        st.append(fz0)

        def fz1():
            k.cp("act", zs.all(), pz.all())
            ta4 = S.sb_at(tas[0].off, [4, 128], F32)
            k.act(ta4.all(), zs.all(), AF.Exp, scale=-1.0)
            k.act(ta4.all(), ta4.all(), AF.Ln, bias=c.epsc[:, 1:2])
        st.append(fz1)

        def fz2():
            ta4 = S.sb_at(tas[0].off, [4, 128], F32)
            tb4 = S.sb_at(tbs[0].off, [4, 128], F32)
            k.act(tb4.all(), ta4.all(), AF.Exp, scale=-1.0)
            k.tt("pool", zs.all(), zs.all(), tb4.all(), ALU.mult)
        st.append(fz2)
        nq = 8
        chains = [st[0:nq], st[nq:2 * nq], st[2 * nq:2 * nq + 6]]
        tail = st[2 * nq + 6:]
        out = []
        for s_ in range(nq):
            for ch in chains:
                if s_ < len(ch):
                    out.append(ch[s_])
        return out + tail

    def unit_steps(h, par):
        qT, kT, vT = qkvs[par]
        zs = zss[par]
        stages = [[] for _ in range(4)]
        for u in range(4):
            b = UB[u]
            t = u
            cols = slice(u * 128, (u + 1) * 128)
            st = stages[u]

            def s_a(b=b, t=t, cols=cols):
                k.tr(b.s3b[:, 0, :], kT[:, cols], c.identb)
                k.tr(b.s3b[:, 1, :], vT[:, cols], c.identb)
                k.ts("dve", b.ke.all(), b.s3b[:, 0, :], egc[:, t, h:h + 1], ALU.mult)
                k.ts("dve", b.kst.all(), b.s3b[:, 0, :], kdec[:, t, h:h + 1], ALU.mult)
                k.cp("act", b.vtok.all(), b.s3b[:, 1, :])
                k.ts("pool", b.G1.all(), c.ones, g_[:, t, h:h + 1], ALU.mult)
            st.append(s_a)

            def s_b(b=b, t=t, cols=cols):
                k.mm(b.s0.all(), b.G1.all(), c.U, start=True, stop=False)
                k.mm(b.s0.all(), c.ident, c.UTm, start=False, stop=True)
                k.mm(b.s12[:, 0:128], kT[:, cols], kT[:, cols])
                k.mm(b.s12[:, 128:256], kT[:, cols], qT[:, cols])
                k.act(b.Dt.all(), b.s0.all(), AF.Exp, bias=ngc[:, t, h:h + 1])
            st.append(s_b)

            def s_c(b=b, t=t):
                k.tt("pool", b.Dts.all(), b.Dt.all(), c.SUP, ALU.mult)
                k.tt("dve", b.attnT.all(), b.s12[:, 128:256], b.Dt.all(), ALU.mult)
                k.stt(b.X[0][:, 128:256], b.s12[:, 0:128], nbeta[:, t, h:h + 1], b.Dts.all(), ALU.mult, ALU.mult)
            st.append(s_c)

            def s_d(b=b):
                k.mm(b.s3.all(), b.X[0][:, 128:256], c.ident)
                k.cp("pool", b.X[0][:, 256:384], c.ident)
                k.cp("dve", b.X[0][:, 0:128], b.s3.all())
            st.append(s_d)
            for lv in range(6):
                def s_e(b=b, lv=lv):
                    X = b.X[lv % 2]
                    k.mm(b.s012[:, 0:128], X[:, 128:256], X[:, 0:128])
                    k.mm(b.s012[:, 128:384], X[:, 0:128], X[:, 128:384])
                    k.mm(b.s012[:, 256:384], c.ident, X[:, 256:384], start=False, stop=True)
                st.append(s_e)

                def s_f(b=b, lv=lv):
                    k.cp("dve", b.X[(lv + 1) % 2].all(), b.s012.all())
                st.append(s_f)

            def s_g(b=b):
                X = b.X[0]
                k.mm(b.s0.all(), X[:, 0:128], X[:, 256:384], start=True, stop=False)
                k.mm(b.s0.all(), c.ident, X[:, 256:384], start=False, stop=True)
                k.cp("act", b.Rtb.all(), b.s0.all())
            st.append(s_g)

            def s_h(b=b):
                k.mm(b.s0.all(), b.ke.all(), b.Rtb.all())
                k.act(b.nkcdT.all(), b.s0.all(), AF.Copy, scale=-1.0)
            st.append(s_h)
        out = []
        for si in range(len(stages[0])):
            for u in range(4):
                out.append(stages[u][si])
        Sh = Sst[:, h, :]
        Shb = Sbf[:, h, :]
        for u in range(4):
            b = UB[u]
            t = u
            cols = slice(u * 128, (u + 1) * 128)

            def q0(b=b, t=t, cols=cols):
                k.mm(b.s1.all(), b.Rtb.all(), b.vtok.all(), start=True, stop=False)
                k.mm(b.s1.all(), b.nkcdT.all(), Shb, start=False, stop=True)
                k.act(b.vnew.all(), b.s1.all(), AF.Copy, scale=beta[:, t, h:h + 1])
                k.mm(b.s0.all(), qT[:, cols], Shb)
            out.append(q0)

            def q1(b=b, t=t, cols=cols):
                k.mm(b.s3.all(), b.kst.all(), b.vnew.all())
                k.mm(b.s2.all(), b.attnT.all(), b.vnew.all())
                k.stt(Sh, Sh, egl[:, t, h:h + 1], b.s3.all(), ALU.mult, ALU.add)
                k.cp("pool", Shb, Sh)
            out.append(q1)

            def q2(b=b, t=t, u=u):
                k.cp("act", b.otmp.all(), b.s2.all())
                k.stt(b.o.all(), b.s0.all(), egc[:, t, h:h + 1], b.otmp.all(), ALU.mult, ALU.add)
                k.act(b.otmp.all(), b.o.all(), AF.Square, accum=ssg[:, u:u + 1])
            out.append(q2)

        def q3():
            k.act(rsg.all(), ssg.all(), AF.Ln, scale=1.0 / 128, bias=c.epsc[:, 0:1])
            k.act(rsg.all(), rsg.all(), AF.Exp, scale=-0.5)
        out.append(q3)
        for u in range(4):
            b = UB[u]

            def q4(b=b, u=u):
                k.stt(b.o.all(), b.o.all(), rsg[:, u:u + 1], gnB.all(), ALU.mult, ALU.mult)
                k.tt("pool", b.og.all(), b.o.all(), zs[:, u, :], ALU.mult)
                k.tr(b.s3b[:, 0, :], b.og.all(), c.identb)
                k.cp("act", oTb[:, h, u * 128:(u + 1) * 128], b.s3b[:, 0, :])
            out.append(q4)
        return out

    load_head(0, wh[0])
    for blk in range(4):
        tiles = list(range(blk * 4, blk * 4 + 4))
        prenorm(c, tiles, c.gB[0], hTb, pT)
        for u in range(4):
            for kc in range(8):
                k.mm(pg[:, u, :], hTb[:, kc, u * 128:(u + 1) * 128], wab[:, kc, :], start=(kc == 0), stop=(kc == 7))
        k.cp("dve", gab.all(), pg.all())
        for h in range(8):
            k.ts("dve", gtmp[:, :, h], gab[:, :, h], dtB[:, h:h + 1], ALU.add)
        k.act(gtmp.all(), gtmp.all(), AF.Exp)
        k.act(gtmp.all(), gtmp.all(), AF.Ln, bias=c.epsc[:, 1:2])
        for h in range(8):
            k.ts("dve", g_[:, :, h], gtmp[:, :, h], negeA[:, h:h + 1], ALU.mult)
        k.act(beta.all(), gab[:, :, 8:16], AF.Exp, scale=-1.0)
        k.act(beta.all(), beta.all(), AF.Ln, bias=c.epsc[:, 1:2])
        k.act(beta.all(), beta.all(), AF.Exp, scale=-1.0)
        k.ts("dve", nbeta.all(), beta.all(), -1.0, ALU.mult)
        for u in range(4):
            k.mm(pgc[:, u, :], c.U, g_[:, u, :])
            k.mm(pgl[:, u, :], c.ones, g_[:, u, :])
        k.cp("dve", gc.all(), pgc.all())
        k.cp("dve", gl.all(), pgl.all())
        k.act(egc.all(), gc.all(), AF.Exp)
        k.ts("dve", ngc.all(), gc.all(), -1.0, ALU.mult)
        k.tt("dve", kdec.all(), gl.all(), gc.all(), ALU.subtract)
        k.act(kdec.all(), kdec.all(), AF.Exp)
        k.act(egl.all(), gl.all(), AF.Exp)

        pend = None
        for h in range(8):
            par = h % 2
            wcur = wh[nwh % 2]
            nwh += 1
            nxt = blk * 8 + h + 1
            if nxt < 32:
                load_head(nxt % 8, wh[nwh % 2])
            A = inproj_steps(h, wcur, par)
            if pend is None:
                for f in A:
                    f()
            else:
                merge_run(pend, A)
            pend = unit_steps(h, par)
        for f in pend:
            f()
        if GSTOP <= 5:
            return
        n_ = 0
        for ch in range(2):
            k.load(woh.all(), WO[:, ch * 512:(ch + 1) * 512].rearrange("(c p) n -> p c n", p=128), eng="pool")
            for u in range(4):
                p = pin[n_ % 2]
                n_ += 1
                for kc in range(8):
                    k.mm(p.all(), oTb[:, kc, u * 128:(u + 1) * 128], woh[:, kc, :], start=(kc == 0), stop=(kc == 7))
                k.cp("act", msb[:, u, ch * 512:(ch + 1) * 512], p.all())
        postnorm_add(c, tiles, lambda t: msb[:, t - tiles[0], :], c.gB[1])


def sigmoid_parts(c, out, x, tmp, scale=1.0):
    k = c.k
    k.act(tmp, x, AF.Exp, scale=-scale)
    k.act(tmp, tmp, AF.Ln, bias=c.epsc[:, 1:2])
    k.act(out, tmp, AF.Exp, scale=-1.0)


def mlstm_layer(c, li):
    S, k = c.S, c.k
    j = li // 3
    W = c.P("mlstm_w_in", j)
    NG = c.P("norm_g", li)
    k.load(c.gB[0].all(), bcast_row(NG[0], D))
    k.load(c.gB[1].all(), bcast_row(NG[1], D))
    wo = S.sb([8, D], BF)
    k.load(wo.all(), c.P("mlstm_w_out", j).rearrange("(c p) n -> p c n", p=128), eng="pool")
    wif = S.sb([8, 16], BF)
    k.load(wif.all(), W[:, 3072:3088].rearrange("(c p) n -> p c n", p=128), eng="pool")
    gbB = S.sb([16], F32)
    k.load(gbB.all(), c.P("mlstm_gate_b", j).rearrange("a b -> (a b)").partition_broadcast(128))
    nrmB = S.sb([D], F32)
    k.load(nrmB.all(), bcast_row(c.P("mlstm_norm_g", j), D))
    Cn = S.sb([8, 132], F32)
    Cnb = S.sb([8, 132], BF)
    mst = S.sb([8], F32)
    k.memset("pool", Cn.all(), 0.0)
    k.memset("pool", Cnb.all(), 0.0)
    k.memset("pool", mst.all(), 0.0)
    hTb = S.sb([8, 512], BF)
    oTb = S.sb([8, 512], BF)
    wh = [S.sb([8, 384], BF), S.sb([8, 384], BF)]
    gab = S.sb([4, 16], F32)
    gt1 = S.sb([4, 16], F32)
    lf = S.sb([4, 8], F32)
    bc = S.sb([4, 8], F32)
    nbc = S.sb([4, 8], F32)
    r_ = S.sb([4, 8], F32)
    ssgs = [S.sb([4], F32) for _ in range(2)]
    rsgs = [S.sb([4], F32) for _ in range(2)]
    hcss = [[S.sb([128], F32) for _ in range(4)] for _ in range(2)]
    off_blk = S.sb_top
    qTs = [S.sb([512], BF) for _ in range(2)]
    kTs = [S.sb([512], BF) for _ in range(2)]
    ktoks = [S.sb([4, 64], F32) for _ in range(2)]
    vaugs = [S.sb([4, 132], BF) for _ in range(2)]
    ogs = [S.sb([4, 128], F32) for _ in range(2)]
    ogt = S.sb([4, 128], F32)
    S.sb_top = max(S.sb_top, off_blk + 4 * D * 4)
    msb = S.sb_at(off_blk, [4, D], F32)
    UB = []
    for u in range(4):
        b = Ctx()
        b.Rb = S.sb([128], F32)
        b.Pm = S.sb([128], F32)
        b.smat = S.sb([128], BF)
        b.sT = S.sb([128], BF)
        b.kw = S.sb([64], BF)
        b.nd = S.sb([132], F32)
        b.tmp = S.sb([132], F32)
        b.hc = S.sb([128], F32)
        b.hb = S.sb([128], BF)
        b.col = S.sb([16], F32)
        base = (4 + u) * 2048
        b.pD0 = S.ps(base, [128], F32)
        b.pQK = S.ps(base + 512, [128], F32)
        b.psT = S.ps(base, [128], BF)
        b.pQC = S.ps(base + 512, [129], F32)
        b.pSV = S.ps(base + 1032, [129], F32)
        b.pdC = S.ps(base, [129], F32)
        b.pmg = S.ps(base + 1552, [2], F32)
        b.pT2 = S.ps(base + 1600, [128], BF)
        UB.append(b)
    pT = S.ps(0, [8, 128], BF)
    pq = S.ps(0, [512], F32)
    pk = S.ps(2048, [512], F32)
    ptok = [S.ps(4096, [320], F32), S.ps(6144, [320], F32)]
    pg = S.ps(6144, [4, 16], F32)
    pbc = S.ps(6144 + 512, [4, 8], F32)
    nwh = 0

    def load_head(hh, buf):
        k.load(buf[:, :, 0:64], W[:, hh * 64:(hh + 1) * 64].rearrange("(c p) n -> p c n", p=128), eng="pool")
        k.load(buf[:, :, 64:128], W[:, 512 + hh * 64:512 + (hh + 1) * 64].rearrange("(c p) n -> p c n", p=128), eng="pool")
        k.load(buf[:, :, 128:256], W[:, 1024 + hh * 128:1024 + (hh + 1) * 128].rearrange("(c p) n -> p c n", p=128), eng="pool")
        k.load(buf[:, :, 256:384], W[:, 2048 + hh * 128:2048 + (hh + 1) * 128].rearrange("(c p) n -> p c n", p=128), eng="pool")


    def merge_run(P, Q):
        n, mq = len(P), len(Q)
        jq = 0
        for i in range(n):
            P[i]()
            tgt = (i + 1) * mq // n
            while jq < tgt:
                Q[jq]()
                jq += 1
        while jq < mq:
            Q[jq]()
            jq += 1

    def inproj_steps(h, wcur, par):
        st = []
        qT, kT, ktok, vaug, og = qTs[par], kTs[par], ktoks[par], vaugs[par], ogs[par]

        def f0():
            for kc in range(8):
                k.mm(pq[0:64, :], wcur[:, kc, 0:64], hTb[:, kc, :], start=(kc == 0), stop=(kc == 7))
            k.act(qT[0:64, :], pq[0:64, :], AF.Copy, scale=0.125)
        st.append(f0)

        def f1():
            for kc in range(8):
                k.mm(pk[0:64, :], wcur[:, kc, 64:128], hTb[:, kc, :], start=(kc == 0), stop=(kc == 7))
            k.cp("dve", kT[0:64, :], pk[0:64, :])
        st.append(f1)
        for u in range(4):
            def f2(u=u):
                p = ptok[u % 2]
                for kc in range(8):
                    k.mm(p.all(), hTb[:, kc, u * 128:(u + 1) * 128], wcur[:, kc, 64:384], start=(kc == 0), stop=(kc == 7))
                k.cp("act", ktok[:, u, :], p[:, 0:64])
                k.cp("dve", vaug[:, u, 0:128], p[:, 64:192])
                k.cp("act", og[:, u, :], p[:, 192:320])
            st.append(f2)

        def f3():
            k.memset("pool", vaug[:, :, 128:129], 1.0)
            k.act(ogt.all(), og.all(), AF.Exp, scale=-1.0)
            k.act(ogt.all(), ogt.all(), AF.Ln, bias=c.epsc[:, 1:2])
        st.append(f3)

        def f4():
            k.act(og.all(), ogt.all(), AF.Exp, scale=-1.0)
        st.append(f4)
        return st

    def unit_steps(h, par, ubs):
        ssg, rsg = ssgs[par], rsgs[par]
        hcs = hcss[par]
        qT, kT, ktok, vaug, og = qTs[par], kTs[par], ktoks[par], vaugs[par], ogs[par]
        out = []
        mcol = mst[:, h:h + 1]
        Ch = Cn[0:64, h, 0:129]
        Chb = Cnb[0:64, h, 0:129]
        pros = []
        for u in range(4):
            b = ubs[u % 2]
            cols = slice(u * 128, (u + 1) * 128)

            def p0(b=b, u=u, cols=cols):
                k.ts("pool", b.Rb.all(), c.ones, r_[:, u, h:h + 1], ALU.mult)
                k.mm(b.pD0.all(), b.Rb.all(), c.ident, start=True, stop=False)
                k.mm(b.pD0.all(), c.ident, c.LTm, start=False, stop=True)
                k.mm(b.pQK.all(), qT[0:64, cols], kT[0:64, cols])
                k.red(b.col[:, 0:1], b.pD0.all(), ALU.max)
            pros.append(p0)
        seqs = []
        for u in range(4):
            b = ubs[u % 2]
            sq_ = []
            cols = slice(u * 128, (u + 1) * 128)
            mx, mm_, nmm, wint, emo, stat, den = (b.col[:, i:i + 1] for i in range(7))
            mnew, gl = b.col[:, 8:9], b.col[:, 9:10]
            gm, wold, kwf = b.col[:, 10:11], b.col[:, 11:12], b.col[:, 12:13]

            def q0(b=b, u=u, mx=mx, mm_=mm_, nmm=nmm, wint=wint, emo=emo):
                k.tt("dve", mm_, mx, mcol, ALU.max)
                k.ts("dve", nmm, mm_, -1.0, ALU.mult)
                k.act(b.Pm.all(), b.pD0.all(), AF.Exp, bias=nmm)
                k.tt("dve", b.smat.all(), b.pQK.all(), b.Pm.all(), ALU.mult)
                k.act(wint, nmm, AF.Exp, bias=mcol)
                k.act(emo, bc[:, u, h:h + 1], AF.Exp, scale=-1.0, bias=nmm)
            sq_.append(q0)

            def q1(b=b, u=u, cols=cols):
                k.tr(b.psT.all(), b.smat.all(), c.identb)
                k.cp("act", b.sT.all(), b.psT.all())
                k.mm(b.pQC[:, 0:129], qT[0:64, cols], Chb)
                k.mm(b.pSV[:, 0:129], b.sT.all(), vaug[:, u, 0:129])
            sq_.append(q1)

            def q2(b=b, u=u, wint=wint, emo=emo, den=den):
                k.cp("act", b.tmp[:, 0:129], b.pSV[:, 0:129])
                k.stt(b.nd[:, 0:129], b.pQC[:, 0:129], wint, b.tmp[:, 0:129], ALU.mult, ALU.add)
                k.ts("dve", den, b.nd[:, 128:129], -1.0, ALU.mult)
                k.tt("dve", den, den, b.nd[:, 128:129], ALU.max)
                k.tt("dve", den, den, emo, ALU.max)
                k.recip(den, den)
                k.ts("dve", hcs[u].all(), b.nd[:, 0:128], den, ALU.mult)
                k.act(b.tmp[:, 0:128], hcs[u].all(), AF.Square, accum=ssg[:, u:u + 1])
            sq_.append(q2)

            def q3(b=b, u=u, stat=stat, mm_=mm_, mnew=mnew, gl=gl, gm=gm, wold=wold, kwf=kwf):
                k.tt("dve", stat, bc[:, u, h:h + 1], mm_, ALU.add)
                k.cp("dve", b.col[:, 7:8], bc[:, u, h:h + 1])
                k.mm(b.pmg[:, 0:1], c.SEL, stat)
                k.mm(b.pmg[:, 1:2], c.SEL, b.col[:, 7:8])
                k.cp("dve", b.col[:, 8:10], b.pmg.all())
                k.tt("dve", gm, gl, mnew, ALU.subtract)
                k.act(wold, gm, AF.Exp, bias=mcol)
                k.act(kwf, r_[:, u, h:h + 1], AF.Exp, bias=gm)
            sq_.append(q3)

            def q4(b=b, u=u, kwf=kwf, mnew=mnew):
                k.ts("dve", b.kw.all(), ktok[:, u, :], kwf, ALU.mult)
                k.mm(b.pdC[0:64, 0:129], b.kw.all(), vaug[:, u, 0:129])
                k.stt(Ch, Ch, b.col[0:64, 11:12], b.pdC[0:64, 0:129], ALU.mult, ALU.add)
                k.cp("pool", Chb, Ch)
                k.cp("dve", mcol, mnew)
            sq_.append(q4)
            seqs.append(sq_)

        out += [pros[0], pros[1]] + seqs[0] + [pros[2]] + seqs[1] + [pros[3]] + seqs[2] + seqs[3]
        def q5():
            k.act(rsg.all(), ssg.all(), AF.Ln, scale=1.0 / 128, bias=c.epsc[:, 0:1])
            k.act(rsg.all(), rsg.all(), AF.Exp, scale=-0.5)
        out.append(q5)
        for u in range(4):
            b = ubs[u % 2]

            def q6(b=b, u=u):
                k.stt(hcs[u].all(), hcs[u].all(), rsg[:, u:u + 1], nrmB[:, h * 128:(h + 1) * 128], ALU.mult, ALU.mult)
                k.tt("pool", b.hb.all(), hcs[u].all(), og[:, u, :], ALU.mult)
                k.tr(b.pT2.all(), b.hb.all(), c.identb)
                k.cp("act", oTb[:, h, u * 128:(u + 1) * 128], b.pT2.all())
            out.append(q6)
        return out

    load_head(0, wh[0])
    for blk in range(4):
        tiles = list(range(blk * 4, blk * 4 + 4))
        prenorm(c, tiles, c.gB[0], hTb, pT)
        for u in range(4):
            for kc in range(8):
                k.mm(pg[:, u, :], hTb[:, kc, u * 128:(u + 1) * 128], wif[:, kc, :], start=(kc == 0), stop=(kc == 7))
        for u in range(4):
            k.tt("dve", gab[:, u, :], pg[:, u, :], gbB.all(), ALU.add)
        sigmoid_parts(c, gab.all(), gab.all(), gt1.all(), scale=2.0 / 15.0)
        k.ts("dve", gab.all(), gab.all(), 30.0, ALU.mult, -15.0, ALU.add)
        k.act(lf.all(), gab[:, :, 8:16], AF.Exp, scale=-1.0)
        k.act(lf.all(), lf.all(), AF.Ln, bias=c.epsc[:, 1:2])
        k.ts("dve", lf.all(), lf.all(), -1.0, ALU.mult)
        for u in range(4):
            k.mm(pbc[:, u, :], c.U, lf[:, u, :])
        k.cp("dve", bc.all(), pbc.all())
        k.ts("dve", nbc.all(), bc.all(), -1.0, ALU.mult)
        k.tt("dve", r_.all(), gab[:, :, 0:8], bc.all(), ALU.subtract)

        for h2 in range(0, 8, 2):
            for par in range(2):
                h = h2 + par
                wcur = wh[nwh % 2]
                nwh += 1
                nxt = blk * 8 + h + 1
                if nxt < 32:
                    load_head(nxt % 8, wh[nwh % 2])
                for f in inproj_steps(h, wcur, par):
                    f()
            A = unit_steps(h2, 0, [UB[0], UB[1]])
            B = unit_steps(h2 + 1, 1, [UB[2], UB[3]])
            for fa, fb in zip(A, B):
                fa()
                fb()
        outproj_post(c, oTb, wo, msb, [pq, pk], tiles, c.gB[1])


def diff_layer(c, li):
    import math
    import os
    S, k = c.S, c.k
    j = li // 3
    lam_init = 0.8 - 0.6 * math.exp(-0.3 * li)
    W = c.P("diff_w_in", j)
    WO = c.P("diff_w_out", j)
    NG = c.P("norm_g", li)
    k.load(c.gB[0].all(), bcast_row(NG[0], D))
    k.load(c.gB[1].all(), bcast_row(NG[1], D))
    kTall = S.sb([8, T], BF)
    vall = S.sb([16, 8, 129], BF)
    hTb = S.sb([8, 512], BF)
    oTb = S.sb([8, 512], BF)
    woh = S.sb([8, 256], BF)
    wh = S.sb([8, 384], BF)
    dmask = S.sb([4, 512], BF)
    ptb = S.sb([128], BF)
    small = S.sb([64], F32)
    gnB = S.sb([128], F32)
    msb = S.sb([4, D], F32)
    lpB = S.sb_at(msb.off, [256], F32)
    ltmp = S.sb_at(msb.off + 1024, [64], F32)
    o0 = msb.off
    q_sb = S.sb_at(o0, [512], BF)
    k_sb = S.sb_at(o0 + 1024, [512], BF)
    qTs = [S.sb_at(o0 + 2048, [512], BF), S.sb_at(o0 + 14592, [512], BF)]
    t1 = S.sb_at(o0 + 3072, [512], F32)
    t2 = S.sb_at(o0 + 5120, [512], F32)
    cs = S.sb_at(o0 + 7168, [2, 512], F32)
    PTt = [S.sb_at(o0 + 11264, [512], BF), S.sb_at(o0 + 12288, [512], BF)]
    ob = S.sb_at(o0 + 13312, [128], F32)
    obt = S.sb_at(o0 + 13824, [128], F32)
    obb = S.sb_at(o0 + 14336, [128], BF)
    zer = S.sb([512], BF)
    pT = S.ps(0, [8, 128], BF)
    pA = S.ps(0, [512], F32)
    pB = S.ps(2048, [512], F32)
    pv = S.ps(2048, [128], F32)
    pT2 = S.ps(2048, [128], BF)
    pS = [S.ps(4096, [512], F32), S.ps(6144, [512], F32)]
    acc = {}
    for cc in range(2):
        for u in range(4):
            bank = 4 + cc * 2 + u // 2
            acc[(cc, u)] = S.ps(bank * 2048 + (u % 2) * 1024, [129], F32)
    accbank = [S.ps((4 + i) * 2048, [512], F32) for i in range(4)]

    k.load(dmask.all(), c.dr["c_dmask"], eng="pool")
    k.memset("pool", ptb.all(), 0.0)
    k.load(ptb[0:64, 0:64], c.dr["c_pt"], eng="pool")
    k.load(ptb[64:128, 64:128], c.dr["c_pt"], eng="pool")
    k.memset("pool", vall[:, :, :, 128:129], 1.0)
    k.memset("pool", zer.all(), 0.0)
    k.load(gnB.all(), bcast_row(c.P("diff_norm_g", j), 128))
    k.ts("dve", gnB.all(), gnB.all(), float(1.0 - lam_init), ALU.mult)
    k.load(lpB.all(), c.P("diff_lambda", j).rearrange("a b -> (a b)").partition_broadcast(128))
    lam = small[:, 0:1]
    k.tt("dve", ltmp.all(), lpB[:, 0:64], lpB[:, 64:128], ALU.mult)
    k.red(small[:, 1:2], ltmp.all(), ALU.add)
    k.tt("dve", ltmp.all(), lpB[:, 128:192], lpB[:, 192:256], ALU.mult)
    k.red(small[:, 2:3], ltmp.all(), ALU.add)
    k.act(small[:, 1:3], small[:, 1:3], AF.Exp)
    k.tt("dve", lam, small[:, 1:2], small[:, 2:3], ALU.subtract)
    k.ts("dve", lam, lam, float(lam_init), ALU.add)
    nps = 0

    for blk in range(4):
        tiles = list(range(blk * 4, blk * 4 + 4))
        prenorm(c, tiles, c.gB[0], hTb, pT)
        k.load(cs.all(), c.dr["c_rope2"][:, :, blk * 512:(blk + 1) * 512])
        def proj_steps(h, par):
            qT = qTs[par]
            st = []

            def g0():
                for i in range(3):
                    k.load(wh[:, :, i * 128:(i + 1) * 128],
                           W[:, i * 1024 + h * 128:i * 1024 + (h + 1) * 128].rearrange("(c p) n -> p c n", p=128), eng="pool")
            st.append(g0)
            for i, (dst_sb, scale) in enumerate(((q_sb, 0.125), (k_sb, 1.0))):
                def g1(i=i, dst_sb=dst_sb, scale=scale):
                    for kc in range(8):
                        k.mm(pA.all(), wh[:, kc, i * 128:(i + 1) * 128], hTb[:, kc, :], start=(kc == 0), stop=(kc == 7))
                    k.act(dst_sb.all(), pA.all(), AF.Copy, scale=scale)
                st.append(g1)

                def g2(i=i, dst_sb=dst_sb):
                    k.mm(pB.all(), ptb.all(), dst_sb.all())
                    k.tt("pool", t1.all(), dst_sb.all(), cs[:, 0, :], ALU.mult)
                    k.tt("dve", t2.all(), pB.all(), cs[:, 1, :], ALU.mult)
                    dst = qT.all() if i == 0 else kTall[:, h, blk * 512:(blk + 1) * 512]
                    k.tt("pool", dst, t1.all(), t2.all(), ALU.add)
                st.append(g2)
            for u in range(4):
                def g3(u=u):
                    for kc in range(8):
                        k.mm(pv.all(), hTb[:, kc, u * 128:(u + 1) * 128], wh[:, kc, 256:384], start=(kc == 0), stop=(kc == 7))
                    k.cp("act", vall[:, blk * 4 + u, h, 0:128], pv.all())
                st.append(g3)
            return st

        def attn_steps(h, par):
            nonlocal nps
            qT = qTs[par]
            st = []

            def a0():
                for i in range(4):
                    k.mm(accbank[i].all(), zer[:, 0:128], zer.all(), start=True, stop=True)
            st.append(a0)
            nk = 4 * blk + 4
            for cc in range(2):
                pr0 = cc * 64
                for jt in range(nk):
                    def a1(cc=cc, pr0=pr0, jt=jt):
                        nonlocal nps
                        p = pS[nps % 2]
                        pt_ = PTt[nps % 2]
                        nps += 1
                        diag = jt - 4 * blk
                        k.mm(p.all(), kTall[pr0:pr0 + 64, h, jt * 128:(jt + 1) * 128], qT[pr0:pr0 + 64, :],
                             start=True, stop=(diag < 0))
                        if diag >= 0:
                            k.mm(p.all(), c.identb, dmask[:, diag, :], start=False, stop=True)
                        k.act(pt_.all(), p.all(), AF.Exp)
                        for u in range(4):
                            if diag > u:
                                continue
                            k.mm(acc[(cc, u)].all(), pt_[:, u * 128:(u + 1) * 128], vall[:, jt, h, 0:129],
                                 start=False, stop=(jt == 4 * blk + u))
                    st.append(a1)
            for u in range(4):
                def a2(u=u):
                    a0_, a1_ = acc[(0, u)], acc[(1, u)]
                    r0, r1 = small[:, 16 + u:17 + u], small[:, 20 + u:21 + u]
                    k.recip(r0, a0_[:, 128:129])
                    k.recip(r1, a1_[:, 128:129])
                    k.tt("dve", r1, r1, lam, ALU.mult)
                    k.ts("dve", obt.all(), a1_[:, 0:128], r1, ALU.mult)
                    k.stt(ob.all(), a0_[:, 0:128], r0, obt.all(), ALU.mult, ALU.subtract)
                    k.act(obt.all(), ob.all(), AF.Square, accum=small[:, 8 + u:9 + u])
                    k.act(small[:, 12 + u:13 + u], small[:, 8 + u:9 + u], AF.Ln, scale=1.0 / 128, bias=c.epsc[:, 0:1])
                    k.act(small[:, 12 + u:13 + u], small[:, 12 + u:13 + u], AF.Exp, scale=-0.5)
                    k.stt(obb.all(), ob.all(), small[:, 12 + u:13 + u], gnB.all(), ALU.mult, ALU.mult)
                    k.tr(pT2.all(), obb.all(), c.identb)
                    k.cp("act", oTb[:, h, u * 128:(u + 1) * 128], pT2.all())
                st.append(a2)
            return st

        def merge_run(P, Q):
            n, mq = len(P), len(Q)
            jq = 0
            for i in range(n):
                P[i]()
                tgt = (i + 1) * mq // n
                while jq < tgt:
                    Q[jq]()
                    jq += 1
            while jq < mq:
                Q[jq]()
                jq += 1

        pend = None
        for h in range(8):
            par = h % 2
            A = proj_steps(h, par)
            if pend is None or os.environ.get("DNOMERGE"):
                if pend is not None:
                    for f in pend:
                        f()
                for f in A:
                    f()
            else:
                merge_run(pend, A)
            pend = attn_steps(h, par)
        for f in pend:
            f()
        n = 0
        for ch in range(4):
            k.load(woh.all(), WO[:, ch * 256:(ch + 1) * 256].rearrange("(c p) n -> p c n", p=128), eng="pool")
            for u in range(4):
                p = [pA, pB][n % 2]
                n += 1
                for kc in range(8):
                    k.mm(p[:, 0:256], oTb[:, kc, u * 128:(u + 1) * 128], woh[:, kc, :], start=(kc == 0), stop=(kc == 7))
                k.cp("act", msb[:, u, ch * 256:(ch + 1) * 256], p[:, 0:256])
        postnorm_add(c, tiles, lambda t: msb[:, t - tiles[0], :], c.gB[1])

FULL_PLAN = [(kind, i) for i in range(4) for kind in ("mix", "mlp")]
_CONSTS = None


def run_plan(inputs, plan, x_override=None, trace=False, ncores=8):
    global _CONSTS
    if _CONSTS is None:
        _CONSTS = make_consts()
    nc = build(plan)
    x = np.ascontiguousarray(np.asarray(inputs["x"] if x_override is None else x_override, dtype=np.float32))
    shared = {key: np.ascontiguousarray(np.asarray(inputs[name], dtype=np.float32)[j]) for key, name, j in nc.needed_params}
    shared.update(_CONSTS)
    in_maps = []
    for b in range(ncores):
        m = dict(shared)
        m["x"] = x[b]
        in_maps.append(m)
    res = run_bass_kernel_spmd(nc, in_maps, core_ids=list(range(ncores)), trace=trace)
    outp = np.stack([np.asarray(res.results[b]["out"], dtype=np.float32) for b in range(ncores)], axis=0)
    return outp, res


def kernel(**inputs):
    outp, _ = run_plan(inputs, FULL_PLAN)
    return outp
```
